# Optimizing a Trainium2 kernel written in Bass

```python
import math
import jax, jax.numpy as jnp
from jax import lax
import numpy as np

D_MODEL = 4096
BATCH = 4
SEQ = 2048
DEPTH = 2

GRID_W = 64
CTX_LEN = 256
HEAD_DIM = 128
N_BRANCH = 4
BRANCH_W = D_MODEL // N_BRANCH
A_HEADS = BRANCH_W // HEAD_DIM
A_KV_HEADS = A_HEADS // 4
A_GROUP = A_HEADS // A_KV_HEADS
S5_P = 16
S5_GROUPS = BRANCH_W // S5_P
S5_N = 64
C_HEADS = BRANCH_W // (2 * HEAD_DIM)
R_HEADS = BRANCH_W // (2 * HEAD_DIM)
R_DV = 2 * HEAD_DIM
RET_CHUNK = 128
Q_BLOCK = 128
ROPE_BASE = 10000.0
ROPE_FREQS = HEAD_DIM // 4
D_FF = 256 * ((8 * D_MODEL // 3 + 255) // 256)
N_EXPERTS = 8
TOP_K = 2
MOE_FF = D_MODEL // 2
N_DENSE = (DEPTH + 1) // 2
N_MOE = DEPTH // 2
EPS = 1e-6
IN_SPLITS = (A_HEADS * HEAD_DIM, A_KV_HEADS * HEAD_DIM, A_KV_HEADS * HEAD_DIM,
             BRANCH_W,
             C_HEADS * 2 * HEAD_DIM, C_HEADS * 2 * HEAD_DIM, C_HEADS * 2 * HEAD_DIM,
             R_HEADS * HEAD_DIM, R_HEADS * HEAD_DIM, R_HEADS * R_DV, BRANCH_W)
IN_COLS = sum(IN_SPLITS)

kernel_name = "hybrid_prefix_flow_trunk"


def rms_norm(x, g):
    xf = x.astype(jnp.float32)
    y = xf * lax.rsqrt(jnp.mean(xf * xf, axis=-1, keepdims=True) + EPS)
    return (y * g.astype(jnp.float32)).astype(x.dtype)


def modulate(x, g, shift, scale):
    return rms_norm(x, g) * (1.0 + scale) + shift


def grid_rope_tables(n_tokens):
    rows = n_tokens // GRID_W
    row = jnp.broadcast_to(jnp.arange(rows)[:, None], (rows, GRID_W)).reshape(-1)
    col = jnp.broadcast_to(jnp.arange(GRID_W)[None, :], (rows, GRID_W)).reshape(-1)
    inv = ROPE_BASE ** (-jnp.arange(ROPE_FREQS, dtype=jnp.float32) / ROPE_FREQS)
    ang = jnp.stack([row, col], axis=-1).astype(jnp.float32)[:, :, None] * inv
    return jnp.cos(ang), jnp.sin(ang)


def apply_rope(x, cos, sin):
    shp = x.shape
    xr = x.astype(jnp.float32).reshape(shp[:-1] + (2, 2, ROPE_FREQS))
    bshape = (shp[1],) + (1,) * (x.ndim - 3) + (2, ROPE_FREQS)
    cs, sn = cos.reshape(bshape), sin.reshape(bshape)
    x1, x2 = xr[..., 0, :], xr[..., 1, :]
    out = jnp.stack([x1 * cs - x2 * sn, x2 * cs + x1 * sn], axis=-2)
    return out.reshape(shp).astype(x.dtype)


def over_query_blocks(f, q):
    n = q.shape[-2]
    nb = n // Q_BLOCK
    qb = jnp.moveaxis(q.reshape(q.shape[:-2] + (nb, Q_BLOCK, q.shape[-1])), -3, 0)
    ob = jnp.moveaxis(lax.map(f, qb), 0, -3)
    return ob.reshape(ob.shape[:-3] + (n, ob.shape[-1]))


def gqa_branch(q_c, k_c, v_c, q_l, k_l, v_l, q_norm, k_norm, cos, sin, need_ctx):
    def heads(t, n):
        return t.reshape(t.shape[:2] + (n, HEAD_DIM))

    def groups(q):
        b, t = q.shape[:2]
        return q.reshape(b, t, A_KV_HEADS, A_GROUP, HEAD_DIM).transpose(0, 2, 3, 1, 4)

    def ungroup(o):
        return o.transpose(0, 3, 1, 2, 4).reshape(o.shape[0], o.shape[3], A_HEADS * HEAD_DIM)

    scale = HEAD_DIM ** -0.5

    def attend(qb, k, v):
        s = jnp.einsum('bkgqd,bskd->bkgqs', qb, k, preferred_element_type=jnp.float32) * scale
        p = jax.nn.softmax(s, axis=-1).astype(v.dtype)
        return jnp.einsum('bkgqs,bskd->bkgqd', p, v)

    qc = rms_norm(heads(q_c, A_HEADS), q_norm)
    kc = rms_norm(heads(k_c, A_KV_HEADS), k_norm)
    vc = heads(v_c, A_KV_HEADS)
    ql = apply_rope(rms_norm(heads(q_l, A_HEADS), q_norm), cos, sin)
    kl = apply_rope(rms_norm(heads(k_l, A_KV_HEADS), k_norm), cos, sin)
    k_all = jnp.concatenate([kc, kl], axis=1)
    v_all = jnp.concatenate([vc, heads(v_l, A_KV_HEADS)], axis=1)
    y_l = ungroup(over_query_blocks(lambda qb: attend(qb, k_all, v_all), groups(ql)))
    y_c = ungroup(attend(groups(qc), kc, vc)) if need_ctx else None
    return y_c, y_l


def _linear_recurrence_combine(e1, e2):
    a1, b1 = e1
    a2, b2 = e2
    return a1 * a2, a2 * b1 + b2


def s5_states(u, a_bar, b_bar, s0, reverse):
    t = u.shape[1]
    bu = jnp.einsum('btgp,gnp->btgn', u.astype(jnp.complex64), b_bar)
    edge = t - 1 if reverse else 0
    bu = bu.at[:, edge].add(a_bar * s0)
    a = jnp.broadcast_to(a_bar, (1, t) + a_bar.shape)
    _, states = lax.associative_scan(_linear_recurrence_combine, (a, bu), reverse=reverse, axis=1)
    return states


def s5_branch(u_c, u_l, a_re, a_im, log_step, b_re, b_im, c_re, c_im, d_skip, w_glu, need_ctx):
    f32 = jnp.float32
    lam = lax.complex(a_re.astype(f32), a_im.astype(f32))
    step = jnp.exp(log_step.astype(f32))[..., None]
    a_bar = jnp.exp(lam * step)
    b_bar = ((a_bar - 1.0) / lam)[..., None] * lax.complex(b_re.astype(f32), b_im.astype(f32))
    c_mat = lax.complex(c_re.astype(f32), c_im.astype(f32))

    def grouped(u):
        return u.astype(f32).reshape(u.shape[:2] + (S5_GROUPS, S5_P))

    uc, ul = grouped(u_c), grouped(u_l)
    zero = jnp.zeros((u_l.shape[0], S5_GROUPS, S5_N), jnp.complex64)
    xc_f = s5_states(uc, a_bar[0], b_bar[0], zero, False)
    xc_b = s5_states(uc, a_bar[1], b_bar[1], zero, True)
    xl_f = s5_states(ul, a_bar[0], b_bar[0], xc_f[:, -1], False)
    xl_b = s5_states(ul, a_bar[1], b_bar[1], xc_b[:, 0], True)

    def finish(x_f, x_b, u):
        y = (jnp.einsum('btgn,gpn->btgp', x_f, c_mat[0]).real
             + jnp.einsum('btgn,gpn->btgp', x_b, c_mat[1]).real)
        y = y.reshape(u.shape) + d_skip.astype(f32) * u.astype(f32)
        z = jax.nn.gelu(y).astype(u.dtype)
        return z * jax.nn.sigmoid(z @ w_glu)

    y_l = finish(xl_f, xl_b, u_l)
    y_c = finish(xc_f, xc_b, u_c) if need_ctx else None
    return y_c, y_l


def diff_branch(q_c, k_c, v_c, q_l, k_l, v_l, lam_params, sub_norm, lam_init, cos, sin, need_ctx):
    def qk_heads(t):
        return t.reshape(t.shape[:2] + (C_HEADS, 2, HEAD_DIM))

    def v_heads(t):
        return t.reshape(t.shape[:2] + (C_HEADS, 2 * HEAD_DIM))

    lp = lam_params.astype(jnp.float32)
    lam = jnp.exp(jnp.sum(lp[0] * lp[1])) - jnp.exp(jnp.sum(lp[2] * lp[3])) + lam_init
    scale = HEAD_DIM ** -0.5

    def attend(qb, k, v):
        s = jnp.einsum('bhmqd,bshmd->bhmqs', qb, k, preferred_element_type=jnp.float32) * scale
        p = jax.nn.softmax(s, axis=-1)
        w = (p[:, :, 0] - lam * p[:, :, 1]).astype(v.dtype)
        return jnp.einsum('bhqs,bshe->bhqe', w, v)

    def finish(o):
        o = rms_norm(o, sub_norm) * (1.0 - lam_init)
        return o.transpose(0, 2, 1, 3).reshape(o.shape[0], o.shape[2], BRANCH_W)

    def to_q(q):
        return q.transpose(0, 2, 3, 1, 4)

    qc, kc, vc = qk_heads(q_c), qk_heads(k_c), v_heads(v_c)
    ql = apply_rope(qk_heads(q_l), cos, sin)
    kl = apply_rope(qk_heads(k_l), cos, sin)
    k_all = jnp.concatenate([kc, kl], axis=1)
    v_all = jnp.concatenate([vc, v_heads(v_l)], axis=1)
    y_l = finish(over_query_blocks(lambda qb: attend(qb, k_all, v_all), to_q(ql)))
    y_c = finish(attend(to_q(qc), kc, vc)) if need_ctx else None
    return y_c, y_l


def retention_scan(q, k, v, log_gamma, s0, need_out):
    b, t, h, dk = q.shape
    nc = t // RET_CHUNK
    f32 = jnp.float32
    qf = q.astype(f32).reshape(b, nc, RET_CHUNK, h, dk)
    kf = k.astype(f32).reshape(b, nc, RET_CHUNK, h, dk)
    vf = v.astype(f32).reshape(b, nc, RET_CHUNK, h, v.shape[-1])
    pos = jnp.arange(RET_CHUNK, dtype=f32)
    k_tail = kf * jnp.exp((RET_CHUNK - 1.0 - pos)[:, None] * log_gamma)[:, :, None]
    chunk_kv = jnp.einsum('bnjhd,bnjhe->nbhde', k_tail, vf)
    chunk_decay = jnp.exp(RET_CHUNK * log_gamma)[:, None, None]

    def step(s, u):
        return chunk_decay * s + u, s

    s_final, s_in = lax.scan(step, s0.astype(f32), chunk_kv)
    if not need_out:
        return None, s_final
    rel = pos[:, None] - pos[None, :]
    dmat = jnp.where(rel >= 0, jnp.exp(jnp.maximum(rel, 0.0)[None] * log_gamma[:, None, None]), 0.0)
    scores = jnp.einsum('bnihd,bnjhd->bnhij', qf, kf) * dmat
    intra = jnp.einsum('bnhij,bnjhe->bnihe', scores, vf)
    q_head = qf * jnp.exp((pos + 1.0)[:, None] * log_gamma)[:, :, None]
    cross = jnp.einsum('bnihd,nbhde->bnihe', q_head, s_in)
    return (intra + cross).reshape(b, t, h, -1), s_final


def retention_branch(q_c, k_c, v_c, g_c, q_l, k_l, v_l, g_l, decay_logit, out_norm, cos, sin, need_ctx):
    def qk_heads(t):
        return t.reshape(t.shape[:2] + (R_HEADS, HEAD_DIM))

    def v_heads(t):
        return t.reshape(t.shape[:2] + (R_HEADS, R_DV))

    kscale = HEAD_DIM ** -0.5
    log_gamma = jax.nn.log_sigmoid(decay_logit.astype(jnp.float32))

    def flip(t):
        return jnp.flip(t, axis=1)

    qc, kc, vc = qk_heads(q_c), qk_heads(k_c) * kscale, v_heads(v_c)
    ql = apply_rope(qk_heads(q_l), cos, sin)
    kl = apply_rope(qk_heads(k_l), cos, sin) * kscale
    vl = v_heads(v_l)
    s0 = jnp.zeros((q_l.shape[0], R_HEADS, HEAD_DIM, R_DV), jnp.float32)
    oc_f, sc_f = retention_scan(qc, kc, vc, log_gamma[0], s0, need_ctx)
    oc_b, sc_b = retention_scan(flip(qc), flip(kc), flip(vc), log_gamma[1], s0, need_ctx)
    ol_f, _ = retention_scan(ql, kl, vl, log_gamma[0], sc_f, True)
    ol_b, _ = retention_scan(flip(ql), flip(kl), flip(vl), log_gamma[1], sc_b, True)

    def finish(o, g):
        y = rms_norm(o, out_norm).astype(g.dtype) * jax.nn.silu(v_heads(g))
        return y.reshape(g.shape)

    y_l = finish(ol_f + flip(ol_b), g_l)
    y_c = finish(oc_f + flip(oc_b), g_c) if need_ctx else None
    return y_c, y_l


def token_mixers(h_c, h_l, cos, sin, lam_init, need_ctx, w_in, q_norm, k_norm,
                 s5_a_re, s5_a_im, s5_log_step, s5_b_re, s5_b_im, s5_c_re, s5_c_im, s5_d, s5_w_glu,
                 diff_lambda, diff_norm, ret_decay_logit, ret_norm, w_branch, w_merge_gate, w_out):
    points = np.cumsum(IN_SPLITS)[:-1].tolist()
    pc = jnp.split(h_c @ w_in, points, axis=-1)
    pl = jnp.split(h_l @ w_in, points, axis=-1)
    ya = gqa_branch(pc[0], pc[1], pc[2], pl[0], pl[1], pl[2], q_norm, k_norm, cos, sin, need_ctx)
    yb = s5_branch(pc[3], pl[3], s5_a_re, s5_a_im, s5_log_step, s5_b_re, s5_b_im, s5_c_re, s5_c_im,
                   s5_d, s5_w_glu, need_ctx)
    yc = diff_branch(pc[4], pc[5], pc[6], pl[4], pl[5], pl[6], diff_lambda, diff_norm, lam_init,
                     cos, sin, need_ctx)
    yd = retention_branch(pc[7], pc[8], pc[9], pc[10], pl[7], pl[8], pl[9], pl[10],
                          ret_decay_logit, ret_norm, cos, sin, need_ctx)

    def merge(h, ys):
        acc = jnp.zeros_like(h)
        for j in range(N_BRANCH):
            acc = acc + jax.nn.sigmoid(h @ w_merge_gate[j]) * (ys[j] @ w_branch[j])
        return acc @ w_out

    y_l = merge(h_l, (ya[1], yb[1], yc[1], yd[1]))
    y_c = merge(h_c, (ya[0], yb[0], yc[0], yd[0])) if need_ctx else None
    return y_c, y_l


def swiglu(h, w1, w3, w2):
    return (jax.nn.silu(h @ w1) * (h @ w3)) @ w2


def moe_swiglu(h, router_w, router_b, w1, w3, w2):
    logits = jnp.einsum('btd,de->bte', h, router_w, preferred_element_type=jnp.float32)
    logits = logits + router_b.astype(jnp.float32)
    top_val, top_idx = lax.top_k(logits, TOP_K)
    top_w = jax.nn.softmax(top_val, axis=-1)
    gate = jnp.einsum('btk,btke->bte', top_w,
                      jax.nn.one_hot(top_idx, N_EXPERTS, dtype=jnp.float32)).astype(h.dtype)
    out = jnp.zeros_like(h)
    for e in range(N_EXPERTS):
        out = out + gate[..., e:e + 1] * swiglu(h, w1[e], w3[e], w2[e])
    return out


def channel_mixer(h, layer, ffn_w1, ffn_w3, ffn_w2, moe_router_w, moe_router_b, moe_w1, moe_w3, moe_w2):
    j = layer // 2
    if layer % 2 == 0:
        return swiglu(h, ffn_w1[j], ffn_w3[j], ffn_w2[j])
    return moe_swiglu(h, moe_router_w[j], moe_router_b[j], moe_w1[j], moe_w3[j], moe_w2[j])


def setup_inputs(seed: int = 0) -> dict:
    key = jax.random.key(seed)
    keys = iter(jax.random.split(key, 48))
    f32 = jnp.float32
    D = D_MODEL
    G, N, P = S5_GROUPS, S5_N, S5_P

    def nrm(shape, scale):
        return jax.random.normal(next(keys), shape, f32) * scale

    def gain(shape):
        return 1.0 + nrm(shape, 0.02)

    ret_base = jnp.log(2.0 ** (5.0 + jnp.arange(R_HEADS, dtype=f32)) - 1.0)
    return {
        'x': nrm((BATCH, SEQ, D), 1.0),
        'c': nrm((BATCH, D), 1.0),
        'ctx': nrm((BATCH, CTX_LEN, D), 1.0),
        'c_ctx': nrm((D,), 1.0),
        'ada_w': nrm((DEPTH, D, 6 * D), 0.4 * D ** -0.5),
        'ada_b': nrm((DEPTH, 6 * D), 0.02),
        'norm1_g': gain((DEPTH, D)),
        'norm2_g': gain((DEPTH, D)),
        'w_in': nrm((DEPTH, D, IN_COLS), D ** -0.5),
        'attn_q_norm': gain((DEPTH, HEAD_DIM)),
        'attn_k_norm': gain((DEPTH, HEAD_DIM)),
        's5_a_re': -0.5 + nrm((DEPTH, 2, G, N), 0.01),
        's5_a_im': math.pi * jnp.arange(N, dtype=f32) + nrm((DEPTH, 2, G, N), 0.01),
        's5_log_step': jax.random.uniform(next(keys), (DEPTH, 2, G), f32, math.log(1e-3), math.log(1e-1)),
        's5_b_re': nrm((DEPTH, 2, G, N, P), (2 * P) ** -0.5),
        's5_b_im': nrm((DEPTH, 2, G, N, P), (2 * P) ** -0.5),
        's5_c_re': nrm((DEPTH, 2, G, P, N), N ** -0.5),
        's5_c_im': nrm((DEPTH, 2, G, P, N), N ** -0.5),
        's5_d': nrm((DEPTH, BRANCH_W), 1.0),
        's5_w_glu': nrm((DEPTH, BRANCH_W, BRANCH_W), BRANCH_W ** -0.5),
        'diff_lambda': nrm((DEPTH, 4, HEAD_DIM), 0.1),
        'diff_norm': gain((DEPTH, 2 * HEAD_DIM)),
        'ret_decay_logit': ret_base + nrm((DEPTH, 2, R_HEADS), 0.01),
        'ret_norm': gain((DEPTH, R_DV)),
        'w_branch': nrm((DEPTH, N_BRANCH, BRANCH_W, D), BRANCH_W ** -0.5),
        'w_merge_gate': nrm((DEPTH, N_BRANCH, D, D), D ** -0.5),
        'w_out': nrm((DEPTH, D, D), D ** -0.5),
        'ffn_w1': nrm((N_DENSE, D, D_FF), D ** -0.5),
        'ffn_w3': nrm((N_DENSE, D, D_FF), D ** -0.5),
        'ffn_w2': nrm((N_DENSE, D_FF, D), D_FF ** -0.5),
        'moe_router_w': nrm((N_MOE, D, N_EXPERTS), D ** -0.5),
        'moe_router_b': nrm((N_MOE, N_EXPERTS), 0.01),
        'moe_w1': nrm((N_MOE, N_EXPERTS, D, MOE_FF), D ** -0.5),
        'moe_w3': nrm((N_MOE, N_EXPERTS, D, MOE_FF), D ** -0.5),
        'moe_w2': nrm((N_MOE, N_EXPERTS, MOE_FF, D), MOE_FF ** -0.5),
        'final_norm_g': gain((D,)),
    }


def reference(x, c, ctx, c_ctx, ada_w, ada_b, norm1_g, norm2_g, w_in, attn_q_norm, attn_k_norm,
              s5_a_re, s5_a_im, s5_log_step, s5_b_re, s5_b_im, s5_c_re, s5_c_im, s5_d, s5_w_glu,
              diff_lambda, diff_norm, ret_decay_logit, ret_norm, w_branch, w_merge_gate, w_out,
              ffn_w1, ffn_w3, ffn_w2, moe_router_w, moe_router_b, moe_w1, moe_w3, moe_w2, final_norm_g):
    cos, sin = grid_rope_tables(x.shape[1])
    x_l, x_c = x, ctx
    for i in range(DEPTH):
        need_ctx = i < DEPTH - 1
        mod_l = jnp.split((jax.nn.silu(c) @ ada_w[i] + ada_b[i])[:, None, :], 6, axis=-1)
        mod_c = jnp.split(jax.nn.silu(c_ctx) @ ada_w[i] + ada_b[i], 6, axis=-1)
        h_l = modulate(x_l, norm1_g[i], mod_l[0], mod_l[1])
        h_c = modulate(x_c, norm1_g[i], mod_c[0], mod_c[1])
        m_c, m_l = token_mixers(h_c, h_l, cos, sin, 0.8 - 0.6 * math.exp(-0.3 * i), need_ctx,
                                w_in[i], attn_q_norm[i], attn_k_norm[i],
                                s5_a_re[i], s5_a_im[i], s5_log_step[i], s5_b_re[i], s5_b_im[i],
                                s5_c_re[i], s5_c_im[i], s5_d[i], s5_w_glu[i],
                                diff_lambda[i], diff_norm[i], ret_decay_logit[i], ret_norm[i],
                                w_branch[i], w_merge_gate[i], w_out[i])
        x_l = x_l + mod_l[2] * m_l
        x_l = x_l + mod_l[5] * channel_mixer(modulate(x_l, norm2_g[i], mod_l[3], mod_l[4]), i,
                                             ffn_w1, ffn_w3, ffn_w2, moe_router_w, moe_router_b,
                                             moe_w1, moe_w3, moe_w2)
        if need_ctx:
            x_c = x_c + mod_c[2] * m_c
            x_c = x_c + mod_c[5] * channel_mixer(modulate(x_c, norm2_g[i], mod_c[3], mod_c[4]), i,
                                                 ffn_w1, ffn_w3, ffn_w2, moe_router_w, moe_router_b,
                                                 moe_w1, moe_w3, moe_w2)
    return rms_norm(x_l, final_norm_g)
```

```python
import math
import numpy as np
import concourse.bass as bass
import concourse.mybir as mybir
from concourse.bass_utils import run_bass_kernel_spmd
from contextlib import ExitStack

F32 = mybir.dt.float32
BF16 = mybir.dt.bfloat16
I32 = mybir.dt.int32
AF = mybir.ActivationFunctionType
ALU = mybir.AluOpType
AX = mybir.AxisListType

D = 4096
KT = 32
NCTX = 256
NLAT = 2048
T = NCTX + NLAT
DEPTH = 2
D_FF = 11008
N_EXP = 8
MOE_FF = 2048
IN_COLS = 8704
EPS = 1e-6
BLOCKS = [(0, 256), (256, 512), (768, 512), (1280, 512), (1792, 512)]
GROUPS = [[0, 1, 2], [3, 4]]
LGROUPS = [[1, 2], [3, 4]]

OB = [(0, 128), (128, 512), (640, 512)]
NOWN = 1152
OWN_SRC = [(0, 128), (256, 1280), (768, 1792)]
EPOCH = 24000
N_DMA_SEMS = 8


def L(name, *args, **kw):
    def f(e):
        return getattr(e, name)(*args, **kw)
    return f


class Buf:
    __slots__ = ("name", "w", "r")

    def __init__(self, name=""):
        self.name = name
        self.w = None
        self.r = {}


class Prog:
    ENGS = ("pe", "act", "dve", "pool", "sp")

    def __init__(self, nc):
        self.nc = nc
        self.q = {e: [] for e in self.ENGS}
        self.cnt = {e: 0 for e in self.ENGS}
        self.known = {e: {} for e in self.ENGS}
        self.dma_rr = {e: 0 for e in ("act", "pool", "sp")}
        self.dma_tgt = {}
        self.sem_keys = set()

    def _need(self, eng, ev, waits):
        if ev is None:
            return
        k, v = ev
        if self.known[eng].get(k, 0) >= v:
            return
        if k[0] == "c":
            for kk, vv in self.known[eng].items():
                if kk[0] == "c" and kk[1] == k[1] and kk[2] > k[2] and vv > 0:
                    return
        self.known[eng][k] = v
        waits[k] = max(waits.get(k, 0), v)

    def _deps(self, eng, reads, writes, skip_pe=False):
        waits = {}
        for b in reads:
            if b.w is not None and not (skip_pe and b.w[0][0] == "c" and b.w[0][1] == "pe"):
                self._need(eng, b.w, waits)
        for b in writes:
            if b.w is not None and not (skip_pe and b.w[0][0] == "c" and b.w[0][1] == "pe"):
                self._need(eng, b.w, waits)
            for k, v in b.r.items():
                if skip_pe and k[0] == "c" and k[1] == "pe":
                    continue
                self._need(eng, (k, v), waits)
        return waits

    def _mark(self, ev, reads, writes):
        k, v = ev
        for b in reads:
            if b.r.get(k, 0) < v:
                b.r[k] = v
        for b in writes:
            b.w = ev
            b.r = {}

    def op(self, eng, fn, reads=(), writes=()):
        waits = self._deps(eng, reads, writes, skip_pe=(eng == "pe"))
        self.cnt[eng] += 1
        n = self.cnt[eng]
        key = ("c", eng, (n - 1) // EPOCH)
        val = (n - 1) % EPOCH + 1
        self.sem_keys.add(key)
        ev = (key, val)
        self.q[eng].append((list(waits.items()), fn, key, 1))
        self._mark(ev, reads, writes)
        return ev

    def dma(self, eng, fn, reads=(), writes=()):
        waits = self._deps(eng, reads, writes)
        i = self.dma_rr[eng]
        self.dma_rr[eng] = (i + 1) % N_DMA_SEMS
        key = ("d", eng, i)
        self.sem_keys.add(key)
        prev = self.dma_tgt.get(key, 0)
        if prev > 0:
            self._need(eng, (key, prev), waits)
        tgt = prev + 16
        self.dma_tgt[key] = tgt
        ev = (key, tgt)
        self.q[eng].append((list(waits.items()), fn, key, 16))
        self._mark(ev, reads, writes)
        return ev

    def coll(self, fn, reads=(), writes=()):
        eng = "pool"
        waits = self._deps(eng, reads, writes)
        key = ("d", "cc", 0)
        self.sem_keys.add(key)
        prev = self.dma_tgt.get(key, 0)
        if prev > 0:
            self._need(eng, (key, prev), waits)
        tgt = prev + 1
        self.dma_tgt[key] = tgt
        ev = (key, tgt)
        self.q[eng].append((list(waits.items()), fn, key, 1))
        self._mark(ev, reads, writes)
        return ev

    def barrier(self):
        evs = []
        for e in self.ENGS:
            n = self.cnt[e]
            if n > 0:
                evs.append((("c", e, (n - 1) // EPOCH), (n - 1) % EPOCH + 1))
        for k, v in self.dma_tgt.items():
            evs.append((k, v))
        for e in self.ENGS:
            waits = {}
            for ev in evs:
                if ev[0][0] == "c" and ev[0][1] == e:
                    continue
                self._need(e, ev, waits)
            if waits:
                self.q[e].append((list(waits.items()), None, None, 0))

    def finish(self):
        self.barrier()
        nc = self.nc
        with ExitStack() as es:
            sems = {}
            for k in sorted(self.sem_keys):
                sems[k] = es.enter_context(nc.semaphore("s_%s_%s_%d" % k))
            block = es.enter_context(nc.Block())
            q = self.q

            def run(engh, lst):
                for waits, fn, key, inc in lst:
                    for k, v in waits:
                        engh.wait_ge(sems[k], v)
                    if fn is not None:
                        fn(engh).then_inc(sems[key], inc)

            @block.tensor
            def _(e):
                run(e, q["pe"])

            @block.scalar
            def _(e):
                run(e, q["act"])

            @block.vector
            def _(e):
                run(e, q["dve"])

            @block.gpsimd
            def _(e):
                run(e, q["pool"])

            @block.sync
            def _(e):
                run(e, q["sp"])


class St:
    def __init__(self, nc):
        self.nc = nc
        self.P = Prog(nc)
        self.base = 16640
        self.off = self.base
        self.limit = 229376 - 64
        self.uid = 0
        self.ps = []
        for i in range(8):
            self.ps.append((nc.alloc_psum_tensor("psb%d" % i, [128, 512], F32), Buf("ps%d" % i)))
        self.ps_rr = 0
        self.dram = {}
        self.dbuf = {}

    def sb(self, shape, dt, name="t"):
        esz = 2 if dt == BF16 else 4
        nbytes = int(np.prod(shape[1:])) * esz
        self.uid += 1
        assert self.off + nbytes <= self.limit, ("SBUF overflow", name, self.off, nbytes)
        t = self.nc.alloc_sbuf_tensor_at("%s_%d" % (name, self.uid), list(shape), dt, offset=self.off)
        self.off += (nbytes + 63) // 64 * 64
        return t, Buf(name)

    def mark(self):
        return self.off

    def sb_rot(self, key, shape, dt, n):
        if not hasattr(self, "rot"):
            self.rot = {}
        if key not in self.rot:
            self.rot[key] = [[self.sb(shape, dt, key) for _ in range(n)], 0]
        ent = self.rot[key]
        r = ent[0][ent[1] % n]
        ent[1] += 1
        return r

    def release(self, m):
        self.P.barrier()
        self.off = m

    def release_keep_rot(self, m):
        self.P.barrier()
        self.off = m
        self.rot = {}

    def psum(self, n=6):
        i = self.ps_rr % n
        self.ps_rr += 1
        return self.ps[i]

    def dt(self, name, shape, dt, kind="Internal"):
        t = self.nc.dram_tensor(name, list(shape), dt, kind=kind).ap()
        self.dram[name] = t
        self.dbuf[name] = Buf(name)
        return t


def load_src(S, src_ap, src_buf, kt0, nkt, group, blocks, name="src"):
    P = S.P
    gtok = sum(blocks[b][1] for b in group)
    t, tb = S.sb([128, nkt, gtok], BF16, name)
    o = 0
    offs = {}
    for b in group:
        t0, n = blocks[b]
        offs[b] = o
        for k0 in range(0, nkt, 8):
            k1 = min(nkt, k0 + 8)
            P.dma("sp", L("dma_start",
                out=t[:, k0:k1, o:o + n],
                in_=src_ap[(kt0 + k0) * 128:(kt0 + k1) * 128, t0:t0 + n].rearrange("(kt p) n -> p kt n", p=128)),
                reads=[src_buf], writes=[tb])
        o += n
    return t, tb, offs


def load_src_sel(S, src_ap, src_buf, kt0, nkt, group, blocks, ysel, name="srcsel"):
    P = S.P
    sel, selb = ysel
    gtok = sum(blocks[b][1] for b in group)
    t, tb = S.sb([128, nkt, gtok], BF16, name)
    o = 0
    offs = {}
    for b in group:
        _, n = blocks[b]
        offs[b] = o
        ta, tab = S.sb_rot("selA", [128, nkt, 512], BF16, 2)
        tbb_, tbbb = S.sb_rot("selB", [128, nkt, 512], BF16, 2)
        for (tt_, ttb, c0) in ((ta, tab, OWN_SRC[b][0]), (tbb_, tbbb, OWN_SRC[b][1])):
            P.dma("sp", L("dma_start", out=tt_[:, :, 0:n],
                          in_=src_ap[kt0 * 128:(kt0 + nkt) * 128, c0:c0 + n].rearrange("(kt p) n -> p kt n", p=128)),
                  reads=[src_buf], writes=[ttb])
        P.op("dve", L("tensor_scalar", t[:, :, o:o + n], ta[:, :, 0:n], sel[:, 0:1], None, ALU.mult), reads=[tab, selb], writes=[tb])
        P.op("dve", L("scalar_tensor_tensor", t[:, :, o:o + n], tbb_[:, :, 0:n], sel[:, 1:2], t[:, :, o:o + n], ALU.mult, ALU.add),
             reads=[tbbb, selb, tb], writes=[tb])
        o += n
    return t, tb, offs


def stream_gemm(S, terms, n_mt, group, blocks, epi, wbufs=4):
    P = S.P
    maxk = max(t["nkt"] for t in terms)
    wts = [S.sb([128, maxk, 128], BF16, "w") for _ in range(wbufs)]
    tiles = [(mt, ti) for mt in range(n_mt) for ti in range(len(terms))]
    PF = wbufs - 1

    def issue(idx):
        mt, ti = tiles[idx]
        term = terms[ti]
        nkt = term["nkt"]
        wt, wb = wts[idx % wbufs]
        wap = term["w"](mt)
        for k0 in range(0, nkt, 16):
            k1 = min(nkt, k0 + 16)
            P.dma("pool", L("dma_start", out=wt[:, k0:k1, :],
                            in_=wap[k0 * 128:k1 * 128, :].rearrange("(kt p) m -> p kt m", p=128)), writes=[wb])

    for idx in range(min(PF, len(tiles))):
        issue(idx)
    for idx, (mt, ti) in enumerate(tiles):
        if idx + PF < len(tiles):
            issue(idx + PF)
        term = terms[ti]
        nkt = term["nkt"]
        wt, wb = wts[idx % wbufs]
        st, sbuf_, offs = term["src"]
        for b in group:
            t0, n = blocks[b]
            o = offs[b]
            ps, pb = S.psum()
            for kt in range(nkt):
                P.op("pe", L("matmul", ps[:, 0:n], wt[:, kt, :], st[:, kt, o:o + n], start=(kt == 0), stop=(kt == nkt - 1)),
                     reads=[wb, sbuf_], writes=[pb])
            epi(ti, mt, b, (t0, n), ps, pb)


def phase_ada(S, ada_w, ada_b, cc, mod, modb):
    P = S.P
    m = S.mark()
    ct_, cb = S.sb([128, KT, 2], F32, "cT")
    for s in range(2):
        P.dma("sp", L("dma_start", out=ct_[:, :, s], in_=cc[s, :].rearrange("(kt p) -> p kt", p=128),
                                                allow_slow_non_contiguous=True), writes=[cb])
    sg, sgb = S.sb([128, KT, 2], F32, "sig")
    cs, csb = S.sb([128, KT, 2], BF16, "silu")
    P.op("act", L("activation", sg[:], ct_[:], AF.Sigmoid), reads=[cb], writes=[sgb])
    P.op("dve", L("tensor_tensor", cs[:], ct_[:], sg[:], ALU.mult), reads=[cb, sgb], writes=[csb])
    bt, bb = S.sb([128, DEPTH, 192], F32, "adab")
    for l in range(ada_b.shape[0]):
        P.dma("sp", L("dma_start", out=bt[:, l, :], in_=ada_b[l, :].rearrange("(ct p) -> p ct", p=128),
                                                allow_slow_non_contiguous=True), writes=[bb])
    WC = 256
    wts = [S.sb([128, KT, WC], BF16, "wada") for _ in range(3)]
    wi = 0
    for l in range(ada_b.shape[0]):
        for c0 in range(0, 6 * D, WC):
            wt, wb = wts[wi % 3]
            wi += 1
            for k0 in range(0, KT, 16):
                P.dma("pool", L("dma_start",
                    out=wt[:, k0:k0 + 16, :],
                    in_=ada_w[l, k0 * 128:(k0 + 16) * 128, c0:c0 + WC].rearrange("(kt p) m -> p kt m", p=128)),
                    writes=[wb])
            for j in range(WC // 128):
                ct = (c0 // 128) + j
                ps, pb = S.psum()
                for kt in range(KT):
                    P.op("pe", L("matmul",
                        ps[:, 0:2], wt[:, kt, j * 128:(j + 1) * 128], cs[:, kt, :], start=(kt == 0), stop=(kt == KT - 1)),
                        reads=[wb, csb], writes=[pb])
                P.op("dve", L("tensor_scalar",
                    mod[:, l, ct, :], ps[:, 0:2], bt[:, l, ct:ct + 1], None, ALU.add),
                    reads=[pb, bb], writes=[modb])
    S.release(m)


def phase_norm(S, x_ap, x_buf, g_ap, mod, modb, layer, which_shift, which_scale, out_ap, out_buf, blocks_sel,
               ones_bf, ones_b, out32_ap=None, out32_buf=None, plain=False, specs=None, out32_off=NCTX):
    P = S.P
    m = S.mark()
    gt, gb = S.sb([128, KT], F32, "g")
    P.dma("sp", L("dma_start", out=gt[:], in_=g_ap.rearrange("(kt p) -> p kt", p=128),
                                      allow_slow_non_contiguous=True), writes=[gb])
    A, Ab = S.sb([128, KT, 2], F32, "A")
    if not plain:
        for s in range(2):
            P.op("dve", L("scalar_tensor_tensor",
                A[:, :, s], mod[:, layer, which_scale * KT:(which_scale + 1) * KT, s], 1.0, gt[:], ALU.add, ALU.mult),
                reads=[modb, gb], writes=[Ab])
    if specs is None:
        specs = []
        for bi in blocks_sel:
            t0_, n_ = BLOCKS[bi]
            specs.append(dict(t0=t0_, n=n_, s=(1 if bi == 0 else 0),
                              loads=[(0, n_, (lambda k0, k1, t0_=t0_, n_=n_: x_ap[k0 * 128:k1 * 128, t0_:t0_ + n_]),
                                      (lambda kts, bi=bi: x_buf(kts, bi)))]))
    for spec in specs:
        t0, n, s = spec["t0"], spec["n"], spec["s"]
        mm = S.mark()
        xt, xb = S.sb([128, KT, n], F32, "x")
        ks = spec.get("kstep", 8)
        for (lo, ln, apfn, bfn) in spec["loads"]:
            for k0 in range(0, KT, ks):
                P.dma("sp", L("dma_start", out=xt[:, k0:k0 + ks, lo:lo + ln],
                              in_=apfn(k0, k0 + ks).rearrange("(kt p) n -> p kt n", p=128)),
                      reads=bfn(range(k0, k0 + ks)), writes=[xb])
        sq, sqb = S.sb([128, KT, n], BF16, "sq")
        P.op("act", L("activation", sq[:], xt[:], AF.Square), reads=[xb], writes=[sqb])
        ps, pb = S.psum()
        for kt in range(KT):
            P.op("pe", L("matmul",
                ps[:, 0:n], ones_bf[:, :], sq[:, kt, :], start=(kt == 0), stop=(kt == KT - 1)),
                reads=[ones_b, sqb], writes=[pb])
        rs, rsb = S.sb([128, n], F32, "rstd")
        P.op("dve", L("tensor_scalar", rs[:], ps[:, 0:n], 1.0 / D, EPS, ALU.mult, ALU.add),
             reads=[pb], writes=[rsb])
        P.op("act", L("activation", rs[:], rs[:], AF.Ln), reads=[rsb], writes=[rsb])
        P.op("act", L("activation", rs[:], rs[:], AF.Exp, scale=-0.5), reads=[rsb], writes=[rsb])
        if plain:
            ho, hob = S.sb([128, KT, n], F32, "ho")
        else:
            ho, hob = S.sb([128, KT, n], BF16, "ho")
        for kt in range(KT):
            eng = "dve" if kt % 2 == 0 else "pool"
            P.op(eng, L("tensor_tensor", xt[:, kt, :], xt[:, kt, :], rs[:], ALU.mult),
                 reads=[xb, rsb], writes=[xb])
            if plain:
                P.op("act", L("activation",
                    ho[:, kt, :], xt[:, kt, :], AF.Copy, scale=gt[:, kt:kt + 1]), reads=[xb, gb], writes=[hob])
            else:
                P.op("act", L("activation",
                    ho[:, kt, :], xt[:, kt, :], AF.Identity, scale=A[:, kt, s:s + 1],
                    bias=mod[:, layer, which_shift * KT + kt, s:s + 1]), reads=[xb, Ab, modb], writes=[hob])
        if plain:
            for k0 in range(0, KT, 8):
                P.dma("sp", L("dma_start",
                    out=out32_ap[k0 * 128:(k0 + 8) * 128, t0 - out32_off:t0 - out32_off + n].rearrange("(kt p) n -> p kt n", p=128),
                    in_=ho[:, k0:k0 + 8, :]), reads=[hob], writes=[out32_buf])
        else:
            for k0 in range(0, KT, 8):
                P.dma("sp", L("dma_start",
                    out=out_ap[k0 * 128:(k0 + 8) * 128, t0:t0 + n].rearrange("(kt p) n -> p kt n", p=128),
                    in_=ho[:, k0:k0 + 8, :]), reads=[hob], writes=[out_buf])
        S.release(mm)
    S.release(m)


def evac_store(S, ps, pb, n, dst_ap, dst_buf, rr=[0], func=None):
    P = S.P
    t, tb = S.sb_rot("ev", [128, 512], BF16, 4)
    rr[0] += 1
    if rr[0] % 2 == 0:
        P.op("act", L("activation", t[:, 0:n], ps[:, 0:n], AF.Copy), reads=[pb], writes=[tb])
    else:
        P.op("dve", L("tensor_copy", t[:, 0:n], ps[:, 0:n]), reads=[pb], writes=[tb])
    P.dma("sp", L("dma_start", out=dst_ap, in_=t[:, 0:n]), reads=[tb], writes=[dst_buf])


def phase_win(S, w_in_l, hT, hTb, projT, projTb):
    for group in GROUPS:
        m = S.mark()
        src = load_src(S, hT, hTb, 0, KT, group, BLOCKS, "hsrc")
        S.rot = {}

        def epi(ti, mt, b, tn, ps, pb):
            t0, n = tn
            evac_store(S, ps, pb, n, projT[mt * 128:(mt + 1) * 128, t0:t0 + n], projTb)
        stream_gemm(S, [dict(w=lambda mt: w_in_l[:, mt * 128:(mt + 1) * 128], nkt=KT, src=src)],
                    IN_COLS // 128, group, BLOCKS, epi)
        S.release(m)


MGROUPS = [[0, 1], [2, 3], [4]]
MLGROUPS = [[1, 2], [3, 4]]


def phase_merge(S, layer, wg_l, wb_l, hT, hTb, yT, yTb, accT, accTb, groups, blocks=None, ysel=None):
    P = S.P
    for group in groups:
        m = S.mark()
        S.rot = {}
        if blocks is None:
            blocks = BLOCKS
        hsrc = load_src(S, hT, hTb, 0, KT, group, blocks, "hsrc")
        if ysel is None:
            ysrc = [load_src(S, yT, yTb, j * 8, 8, group, blocks, "ysrc%d" % j) for j in range(4)]
        else:
            ysrc = [load_src_sel(S, yT, yTb, j * 8, 8, group, blocks, ysel, "ysrc%d" % j) for j in range(4)]
        gtok = sum(blocks[b][1] for b in group)
        acc, accb = S.sb([128, gtok], F32, "acc")
        terms = []
        for j in range(4):
            terms.append(dict(w=lambda mt, j=j: wg_l[j, :, mt * 128:(mt + 1) * 128], nkt=KT, src=hsrc))
            terms.append(dict(w=lambda mt, j=j: wb_l[j, :, mt * 128:(mt + 1) * 128], nkt=8, src=ysrc[j]))
        sigs = {}

        def epi(ti, mt, b, tn, ps, pb):
            t0, n = tn
            o = hsrc[2][b]
            j = ti // 2
            if ti % 2 == 0:
                sg, sgb = S.sb_rot("sig%d" % b, [128, 512], F32, 2)
                sigs[b] = (sg, sgb)
                P.op("act", L("activation", sg[:, 0:n], ps[:, 0:n], AF.Sigmoid), reads=[pb], writes=[sgb])
            else:
                sg, sgb = sigs[b]
                if j == 0:
                    P.op("dve", L("tensor_tensor", acc[:, o:o + n], sg[:, 0:n], ps[:, 0:n], ALU.mult),
                         reads=[sgb, pb], writes=[accb])
                else:
                    P.op("dve", L("tensor_tensor", sg[:, 0:n], sg[:, 0:n], ps[:, 0:n], ALU.mult),
                         reads=[sgb, pb], writes=[sgb])
                    P.op("pool", L("tensor_tensor", acc[:, o:o + n], acc[:, o:o + n], sg[:, 0:n], ALU.add),
                         reads=[sgb, accb], writes=[accb])
                if j == 3:
                    t, tb = S.sb_rot("accbf", [128, 512], BF16, 3)
                    P.op("act", L("activation", t[:, 0:n], acc[:, o:o + n], AF.Copy), reads=[accb], writes=[tb])
                    P.dma("sp", L("dma_start", out=accT[mt * 128:(mt + 1) * 128, t0:t0 + n], in_=t[:, 0:n]),
                          reads=[tb], writes=[accTb])
        stream_gemm(S, terms, KT, group, blocks, epi, wbufs=3)
        S.release(m)


def resid_epi(S, layer, which_gate, mod, modb, xs, xbufs):
    P = S.P

    def epi(ti, mt, b, tn, ps, pb):
        t0, n = tn
        s = 1 if b == 0 else 0
        xt, xtb = S.sb_rot("xres", [128, 512], F32, 3)
        xb = xbufs[(mt, b)]
        P.dma("act", L("dma_start", out=xt[:, 0:n], in_=xs[mt * 128:(mt + 1) * 128, t0:t0 + n]),
              reads=[xb], writes=[xtb])
        P.op("dve", L("scalar_tensor_tensor",
            xt[:, 0:n], ps[:, 0:n], mod[:, layer, which_gate * KT + mt, s:s + 1], xt[:, 0:n], ALU.mult, ALU.add),
            reads=[pb, modb, xtb], writes=[xtb])
        P.dma("sp", L("dma_start", out=xs[mt * 128:(mt + 1) * 128, t0:t0 + n], in_=xt[:, 0:n]),
              reads=[xtb], writes=[xb])
    return epi


def phase_proj_resid(S, layer, which_gate, w_fn, K_total, srcT, srcTb, mod, modb, xs, xbufs, groups, kchunk, blocks=None):
    nkt_total = K_total // 128
    for k0 in range(0, nkt_total, kchunk):
        nk = min(kchunk, nkt_total - k0)
        for group in groups:
            m = S.mark()
            S.rot = {}
            src = load_src(S, srcT, srcTb, k0, nk, group, blocks or BLOCKS, "psrc")
            epi = resid_epi(S, layer, which_gate, mod, modb, xs, xbufs)
            stream_gemm(S, [dict(w=lambda mt, k0=k0, nk=nk: w_fn(k0 * 128, (k0 + nk) * 128, mt), nkt=nk, src=src)],
                        KT, group, blocks or BLOCKS, epi, wbufs=3)
            S.release(m)


def phase_ffn_hidden(S, w1, w3, n_ft, hT, hTb, hidT, hidTb, groups, gate_rows=None, blocks=None, gate_off=NCTX):
    P = S.P
    for group in groups:
        m = S.mark()
        S.rot = {}
        src = load_src(S, hT, hTb, 0, KT, group, blocks or BLOCKS, "fsrc")
        sgs = {}

        def epi(ti, mt, b, tn, ps, pb):
            t0, n = tn
            if ti == 0:
                sg, sgb = S.sb_rot("fsig%d" % b, [128, 512], F32, 2)
                sgs[b] = (sg, sgb)
                P.op("act", L("activation", sg[:, 0:n], ps[:, 0:n], AF.Silu), reads=[pb], writes=[sgb])
            else:
                sg, sgb = sgs[b]
                t, tb = S.sb_rot("fhid", [128, 512], BF16, 3)
                if gate_rows is None:
                    P.op("dve", L("tensor_tensor", t[:, 0:n], sg[:, 0:n], ps[:, 0:n], ALU.mult),
                         reads=[sgb, pb], writes=[tb])
                else:
                    gr, grb = gate_rows
                    P.op("dve", L("tensor_tensor", sg[:, 0:n], sg[:, 0:n], ps[:, 0:n], ALU.mult),
                         reads=[sgb, pb], writes=[sgb])
                    P.op("pool", L("tensor_tensor", t[:, 0:n], sg[:, 0:n], gr[:, t0 - gate_off:t0 - gate_off + n], ALU.mult),
                         reads=[sgb, grb], writes=[tb])
                P.dma("sp", L("dma_start", out=hidT[mt * 128:(mt + 1) * 128, t0:t0 + n], in_=t[:, 0:n]),
                      reads=[tb], writes=[hidTb])
        stream_gemm(S, [dict(w=lambda mt: w1[:, mt * 128:(mt + 1) * 128], nkt=KT, src=src),
                        dict(w=lambda mt: w3[:, mt * 128:(mt + 1) * 128], nkt=KT, src=src)],
                    n_ft, group, blocks or BLOCKS, epi, wbufs=4)
        S.release(m)


PARAM_SHAPES = {
    'ada_w': [DEPTH, D, 6 * D], 'ada_b': [DEPTH, 6 * D], 'norm1_g': [DEPTH, D], 'norm2_g': [DEPTH, D],
    'w_in': [DEPTH, D, IN_COLS], 'attn_q_norm': [DEPTH, 128], 'attn_k_norm': [DEPTH, 128],
    's5_a_re': [DEPTH, 2, 64, 64], 's5_a_im': [DEPTH, 2, 64, 64], 's5_log_step': [DEPTH, 2, 64],
    's5_b_re': [DEPTH, 2, 64, 64, 16], 's5_b_im': [DEPTH, 2, 64, 64, 16],
    's5_c_re': [DEPTH, 2, 64, 16, 64], 's5_c_im': [DEPTH, 2, 64, 16, 64],
    's5_d': [DEPTH, 1024], 's5_w_glu': [DEPTH, 1024, 1024], 'diff_lambda': [DEPTH, 4, 128],
    'diff_norm': [DEPTH, 256], 'ret_decay_logit': [DEPTH, 2, 4], 'ret_norm': [DEPTH, 256],
    'w_branch': [DEPTH, 4, 1024, D], 'w_merge_gate': [DEPTH, 4, D, D], 'w_out': [DEPTH, D, D],
    'ffn_w1': [1, D, D_FF], 'ffn_w3': [1, D, D_FF], 'ffn_w2': [1, D_FF, D],
    'moe_router_w': [1, D, N_EXP], 'moe_router_b': [1, N_EXP],
    'moe_w1': [1, N_EXP, D, MOE_FF], 'moe_w3': [1, N_EXP, D, MOE_FF], 'moe_w2': [1, N_EXP, MOE_FF, D],
    'final_norm_g': [D],
}


def build(cfg=None):
    cfg = cfg or {}
    dump = set(cfg.get("dump", ()))
    stop = cfg.get("stop")
    nc = bass.Bass("TRN2", target_bir_lowering=False)
    S = St(nc)
    P = S.P

    def ext(name, shape, dt=F32):
        return nc.dram_tensor(name, list(shape), dt, kind="ExternalInput").ap()

    def scr(name, shape, dt):
        return S.dt(name, shape, dt, kind=("ExternalOutput" if name in dump else "Internal"))

    xT_in = ext("xT", [D, T])
    xTo_in = ext("xTo", [D, NOWN])
    psel_in = ext("pairsel", [128, 2])
    cc = ext("cc", [2, D])
    depth = cfg.get("depth", DEPTH)
    W = {}
    for k, v in PARAM_SHAPES.items():
        v = list(v)
        if k.startswith("moe") and depth < 2:
            continue
        if v[0] == DEPTH and k != 'final_norm_g' and len(v) > 1:
            v[0] = depth
        W[k] = ext(k, v)
    CONSTS = {k: ext(k, list(v.shape)) for k, v in host_consts().items()}
    S.consts = CONSTS
    outT = nc.dram_tensor("outT", [D, NLAT // 2], F32, kind="ExternalOutput").ap()
    outTb = Buf("outT")
    xso = scr("xso", [D, NOWN], F32)
    NCH = 16
    xsc = [scr("xsc%d" % ch, [256, NOWN], F32) for ch in range(NCH)]
    xgc = [scr("xgc%d" % ch, [512, NOWN], F32) for ch in range(NCH)]
    xgb = Buf("xg")
    hT = scr("hT", [D, T], BF16)
    hTo = scr("hTo", [D, NOWN], BF16)
    projT = scr("projT", [IN_COLS, T], BF16)
    yT = scr("yT", [D, T], BF16)
    yTo = scr("yTo", [D, NOWN], BF16)
    accTo = scr("accTo", [D, NOWN], BF16)
    hidTo = scr("hidTo", [D_FF, NOWN], BF16)
    xobufs = {(mt, b): Buf("xo%d_%d" % (mt, b)) for mt in range(KT) for b in range(len(OB))}

    mod, modb = S.sb([128, DEPTH, 192, 2], F32, "mod")
    ones_bf, ones_b = S.sb([128, 128], BF16, "ones")
    P.op("dve", L("memset", ones_bf[:], 1.0), writes=[ones_b])
    psel, pselb = S.sb([128, 2], F32, "psel")
    P.dma("sp", L("dma_start", out=psel[:], in_=psel_in), writes=[pselb])
    S.psel = (psel, pselb)
    S.ncores = cfg.get("ncores", 8)

    for bi, (t0, n) in enumerate(OB):
        for k0 in range(0, KT, 8):
            P.dma("sp", L("dma_start", out=xso[k0 * 128:(k0 + 8) * 128, t0:t0 + n], in_=xTo_in[k0 * 128:(k0 + 8) * 128, t0:t0 + n]),
                  writes=[xobufs[(kt, bi)] for kt in range(k0, k0 + 8)])

    def done():
        P.finish()
        return nc

    phase_ada(S, W['ada_w'], W['ada_b'], cc, mod, modb)
    if stop == "ada":
        return done()

    def own_specs(bsel):
        sp = []
        for bi in bsel:
            c0, n = OB[bi]
            sp.append(dict(t0=c0, n=n, s=(1 if bi == 0 else 0),
                           loads=[(0, n, (lambda k0, k1, c0=c0, n=n: xso[k0 * 128:k1 * 128, c0:c0 + n]),
                                   (lambda kts, bi=bi: [xobufs[(kt, bi)] for kt in kts]))]))
        return sp

    def full_specs(layer):
        sp = []
        if layer == 0:
            for bi, (t0, n) in enumerate(BLOCKS):
                sp.append(dict(t0=t0, n=n, s=(1 if bi == 0 else 0),
                               loads=[(0, n, (lambda k0, k1, t0=t0, n=n: xT_in[k0 * 128:k1 * 128, t0:t0 + n]), (lambda kts: []))]))
        else:
            def gsrc(r, c0, n):
                return lambda k0, k1: xgc[k0 // 2][r * 256:(r + 1) * 256, c0:c0 + n]
            sp.append(dict(t0=0, n=NCTX, s=1, kstep=2, loads=[
                (0, 128, gsrc(0, 0, 128), (lambda kts: [xgb])),
                (128, 128, gsrc(1, 0, 128), (lambda kts: [xgb]))]))
            for i in range(4):
                r, ob = i // 2, 1 + i % 2
                c0 = OB[ob][0]
                sp.append(dict(t0=NCTX + i * 512, n=512, s=0, kstep=2, loads=[
                    (0, 512, gsrc(r, c0, 512), (lambda kts: [xgb]))]))
        return sp

    for layer in range(depth):
        need_ctx = layer < DEPTH - 1
        oblk = [0, 1, 2] if need_ctx else [1, 2]
        ogroups = [oblk]
        phase_norm(S, None, None, W['norm1_g'][layer], mod, modb, layer, 0, 1, hT, S.dbuf["hT"], None, ones_bf, ones_b,
                   specs=full_specs(layer))
        phase_norm(S, None, None, W['norm1_g'][layer], mod, modb, layer, 0, 1, hTo, S.dbuf["hTo"], None, ones_bf, ones_b,
                   specs=own_specs(oblk))
        if stop == "L%d_norm1" % layer:
            return done()
        phase_win(S, W['w_in'][layer], hT, S.dbuf["hT"], projT, S.dbuf["projT"])
        if stop == "L%d_win" % layer:
            return done()
        phase_mixers(S, layer, W, projT, S.dbuf["projT"], yT, S.dbuf["yT"], need_ctx, cfg, nc, dump)
        if stop == "L%d_mix" % layer:
            return done()
        phase_ysel(S, yT, S.dbuf["yT"], yTo, S.dbuf["yTo"], oblk, psel, pselb)
        phase_merge(S, layer, W['w_merge_gate'][layer], W['w_branch'][layer], hTo, S.dbuf["hTo"], yTo, S.dbuf["yTo"],
                    accTo, S.dbuf["accTo"], ogroups, blocks=OB)
        if stop == "L%d_merge" % layer:
            return done()
        wo = W['w_out'][layer]
        phase_proj_resid(S, layer, 2, lambda r0, r1, mt, wo=wo: wo[r0:r1, mt * 128:(mt + 1) * 128], D,
                         accTo, S.dbuf["accTo"], mod, modb, xso, xobufs, ogroups, KT, blocks=OB)
        phase_norm(S, None, None, W['norm2_g'][layer], mod, modb, layer, 3, 4, hTo, S.dbuf["hTo"], None, ones_bf, ones_b,
                   specs=own_specs(oblk))
        if layer % 2 == 0:
            j = layer // 2
            phase_ffn_hidden(S, W['ffn_w1'][j], W['ffn_w3'][j], D_FF // 128, hTo, S.dbuf["hTo"], hidTo, S.dbuf["hidTo"],
                             ogroups, blocks=OB)
            w2 = W['ffn_w2'][j]
            phase_proj_resid(S, layer, 5, lambda r0, r1, mt, w2=w2: w2[r0:r1, mt * 128:(mt + 1) * 128], D_FF,
                             hidTo, S.dbuf["hidTo"], mod, modb, xso, xobufs, ogroups, 43, blocks=OB)
        else:
            phase_moe(S, layer, W, hTo, S.dbuf["hTo"], hidTo, S.dbuf["hidTo"], mod, modb, xso, xobufs, ogroups, nc, cfg, dump)
        if stop == "L%d" % layer:
            if "xso" in dump:
                pass
            return done()
        if layer == 0 and depth > 1:
            groups_cc = [[2 * i, 2 * i + 1] for i in range(4)]
            for ch in range(NCH):
                scb = S.dbuf["xsc%d" % ch]
                P.dma("sp", L("dma_start", out=xsc[ch], in_=xso[ch * 256:(ch + 1) * 256, :]),
                      reads=[xobufs[(kt, b)] for kt in (2 * ch, 2 * ch + 1) for b in range(len(OB))], writes=[scb])
                P.coll(L("collective_compute", "AllGather", ALU.bypass, replica_groups=groups_cc,
                         ins=[xsc[ch].opt()], outs=[xgc[ch].opt()]), reads=[scb], writes=[xgb])
    phase_norm(S, None, None, W['final_norm_g'], mod, modb, 0, 0, 0, None, None, None, ones_bf, ones_b,
               out32_ap=outT, out32_buf=outTb, plain=True, specs=own_specs([1, 2]), out32_off=128)
    return done()


def phase_ysel(S, yT, yTb, yTo, yTob, oblk, sel, selb):
    P = S.P
    m = S.mark()
    S.rot = {}
    for b in oblk:
        c0, n = OB[b]
        for k0 in range(0, KT, 8):
            ta, tab = S.sb_rot("selA", [128, 8, 512], BF16, 2)
            tb_, tbb = S.sb_rot("selB", [128, 8, 512], BF16, 2)
            to, tob = S.sb_rot("selO", [128, 8, 512], BF16, 2)
            for (tt_, ttb, cc0) in ((ta, tab, OWN_SRC[b][0]), (tb_, tbb, OWN_SRC[b][1])):
                P.dma("sp", L("dma_start", out=tt_[:, :, 0:n],
                              in_=yT[k0 * 128:(k0 + 8) * 128, cc0:cc0 + n].rearrange("(kt p) n -> p kt n", p=128)),
                      reads=[yTb], writes=[ttb])
            P.op("dve", L("tensor_scalar", to[:, :, 0:n], ta[:, :, 0:n], sel[:, 0:1], None, ALU.mult), reads=[tab, selb], writes=[tob])
            P.op("dve", L("scalar_tensor_tensor", to[:, :, 0:n], tb_[:, :, 0:n], sel[:, 1:2], to[:, :, 0:n], ALU.mult, ALU.add),
                 reads=[tbb, selb, tob], writes=[tob])
            P.dma("sp", L("dma_start", out=yTo[k0 * 128:(k0 + 8) * 128, c0:c0 + n].rearrange("(kt p) n -> p kt n", p=128),
                          in_=to[:, :, 0:n]), reads=[tob], writes=[yTob])
    S.release(m)
    S.rot = {}


def phase_mixers(S, layer, W, projT, projTb, yT, yTb, need_ctx, cfg, nc, dump):
    m = S.mark()
    S.rot = {}
    M = mix_setup(S, nc, S.consts)
    which = cfg.get("mixers", "abcd")
    if "a" in which:
        mixer_gqa(S, M, W, layer, projT, projTb, yT, yTb, need_ctx)
    if "c" in which:
        mixer_diff(S, M, W, layer, projT, projTb, yT, yTb, need_ctx)
    if "d" in which:
        mixer_ret(S, M, W, layer, projT, projTb, yT, yTb, need_ctx)
    if "b" in which:
        mixer_s5(S, M, W, layer, projT, projTb, yT, yTb, need_ctx)
    S.release(m)
    S.rot = {}


def phase_moe(S, layer, W, hT, hTb, hidT, hidTb, mod, modb, xs, xbufs, fgroups, nc, cfg, dump):
    P = S.P
    j = layer // 2
    m = S.mark()
    S.rot = {}
    ident, identb = S.sb([128, 128], BF16, "mident")
    P.dma("pool", L("dma_start", out=ident[:], in_=S.consts["ident"]), writes=[identb])
    identf, identfb = S.sb([128, 128], F32, "midentf")
    P.dma("sp", L("dma_start", out=identf[:], in_=S.consts["ident"]), writes=[identfb])
    sel, selb = S.sb([8, N_EXP * 128], F32, "sel")
    P.dma("sp", L("dma_start", out=sel[:], in_=S.consts["sel"]), writes=[selb])
    rw, rwb = S.sb([128, KT, N_EXP], BF16, "rw")
    P.dma("pool", L("dma_start", out=rw[:], in_=W['moe_router_w'][j].rearrange("(kt p) e -> p kt e", p=128)), writes=[rwb])
    rbias, rbiasb = bcast_row(S, W['moe_router_b'][j], N_EXP, "rbias")
    NOL = NLAT // 2
    gT, gTb = S.sb([8, NOL], F32, "gT")
    for tt in range(NOL // 128):
        t0 = 128 + tt * 128
        mm = S.mark()
        hs, hsb = S.sb([128, KT, 128], BF16, "rh")
        for k0 in range(0, KT, 8):
            P.dma("sp", L("dma_start", out=hs[:, k0:k0 + 8, :],
                          in_=hT[k0 * 128:(k0 + 8) * 128, t0:t0 + 128].rearrange("(kt p) n -> p kt n", p=128)),
                  reads=[hTb], writes=[hsb])
        ps, pb = S.psum()
        for kt in range(KT):
            P.op("pe", L("matmul", ps[:, 0:N_EXP], hs[:, kt, :], rw[:, kt, :], start=(kt == 0), stop=(kt == KT - 1)),
                 reads=[hsb, rwb], writes=[pb])
        lg, lgb = S.sb([128, N_EXP], F32, "lgt")
        P.op("dve", L("tensor_tensor", lg[:], ps[:, 0:N_EXP], rbias[:], ALU.add), reads=[pb, rbiasb], writes=[lgb])
        mx, mxb = S.sb([128, 4], F32, "mx")
        mk1, mk1b = S.sb([128, N_EXP], F32, "mk1")
        l2, l2b = S.sb([128, N_EXP], F32, "l2")
        mk2, mk2b = S.sb([128, N_EXP], F32, "mk2")
        P.op("dve", L("reduce_max", mx[:, 0:1], lg[:], AX.X), reads=[lgb], writes=[mxb])
        P.op("dve", L("tensor_scalar", mk1[:], lg[:], mx[:, 0:1], None, ALU.is_ge), reads=[lgb, mxb], writes=[mk1b])
        P.op("dve", L("scalar_tensor_tensor", l2[:], mk1[:], -1e30, lg[:], ALU.mult, ALU.add), reads=[mk1b, lgb], writes=[l2b])
        P.op("dve", L("reduce_max", mx[:, 1:2], l2[:], AX.X), reads=[l2b], writes=[mxb])
        P.op("dve", L("tensor_scalar", mk2[:], l2[:], mx[:, 1:2], None, ALU.is_ge), reads=[l2b, mxb], writes=[mk2b])
        P.op("dve", L("tensor_tensor", mx[:, 2:3], mx[:, 1:2], mx[:, 0:1], ALU.subtract), reads=[mxb], writes=[mxb])
        P.op("act", L("activation", mx[:, 2:3], mx[:, 2:3], AF.Exp), reads=[mxb], writes=[mxb])
        P.op("dve", L("tensor_scalar", mx[:, 3:4], mx[:, 2:3], 1.0, None, ALU.add), reads=[mxb], writes=[mxb])
        P.op("dve", L("reciprocal", mx[:, 3:4], mx[:, 3:4]), reads=[mxb], writes=[mxb])
        P.op("dve", L("tensor_tensor", mx[:, 2:3], mx[:, 2:3], mx[:, 3:4], ALU.mult), reads=[mxb], writes=[mxb])
        P.op("dve", L("tensor_scalar", mk1[:], mk1[:], mx[:, 3:4], None, ALU.mult), reads=[mk1b, mxb], writes=[mk1b])
        P.op("dve", L("scalar_tensor_tensor", mk1[:], mk2[:], mx[:, 2:3], mk1[:], ALU.mult, ALU.add),
             reads=[mk2b, mxb, mk1b], writes=[mk1b])
        ps2, pb2 = S.psum()
        P.op("pe", L("matmul", ps2[0:8, 0:128], mk1[:], identf[:, :], start=True, stop=True),
             reads=[mk1b, identfb], writes=[pb2])
        P.op("act", L("activation", gT[:, tt * 128:(tt + 1) * 128], ps2[0:8, 0:128], AF.Copy), reads=[pb2], writes=[gTb])
        S.release(mm)
    if "gT" in dump:
        go = nc.dram_tensor("gT_o", [8, NOL], F32, kind="ExternalOutput").ap()
        P.dma("sp", L("dma_start", out=go, in_=gT[:]), reads=[gTb])
    grow, growb = S.sb([128, NOL], F32, "grow")
    for ex in range(N_EXP):
        for c0 in range(0, NOL, 512):
            ps, pb = S.psum()
            P.op("pe", L("matmul", ps[:, 0:512], sel[:, ex * 128:(ex + 1) * 128], gT[:, c0:c0 + 512], start=True, stop=True),
                 reads=[selb, gTb], writes=[pb])
            P.op("act", L("activation", grow[:, c0:c0 + 512], ps[:, 0:512], AF.Copy), reads=[pb], writes=[growb])
        phase_ffn_hidden(S, W['moe_w1'][j, ex], W['moe_w3'][j, ex], MOE_FF // 128, hT, hTb, hidT, hidTb, fgroups,
                         gate_rows=(grow, growb), blocks=OB, gate_off=128)
        w2 = W['moe_w2'][j, ex]
        phase_proj_resid(S, layer, 5, lambda r0, r1, mt, w2=w2: w2[r0:r1, mt * 128:(mt + 1) * 128], MOE_FF,
                         hidT, hidTb, mod, modb, xs, xbufs, fgroups, 16, blocks=OB)
    S.release(m)
    S.rot = {}


def make_in_maps(inputs, n_cores=8, depth=DEPTH):
    x = np.asarray(inputs['x'], np.float32)
    c = np.asarray(inputs['c'], np.float32)
    ctx = np.asarray(inputs['ctx'], np.float32)
    c_ctx = np.asarray(inputs['c_ctx'], np.float32)
    shared = {}
    for k, v in PARAM_SHAPES.items():
        if k.startswith("moe") and depth < 2:
            continue
        a = np.asarray(inputs[k], np.float32)
        if v[0] == DEPTH and k != 'final_norm_g' and len(v) > 1 and depth < DEPTH:
            a = a[:depth]
        shared[k] = np.ascontiguousarray(a)
    shared.update(host_consts())
    maps = []
    for core in range(n_cores):
        b, r = core // 2, core % 2
        xT = np.ascontiguousarray(np.concatenate([ctx[b], x[b]], axis=0).T)
        xTo = np.ascontiguousarray(np.concatenate([ctx[b][r * 128:(r + 1) * 128], x[b][r * 1024:(r + 1) * 1024]], axis=0).T)
        cc = np.ascontiguousarray(np.stack([c[b], c_ctx], axis=0))
        m = dict(shared)
        m["xT"] = xT
        m["xTo"] = xTo
        m["cc"] = cc
        ps = np.zeros((128, 2), np.float32)
        ps[:, r] = 1.0
        m["pairsel"] = ps
        maps.append(m)
    return maps


def kernel(**inputs):
    nc = build()
    maps = make_in_maps(inputs, 8)
    res = run_bass_kernel_spmd(nc, maps, core_ids=list(range(8)))
    out = np.zeros((4, NLAT, D), np.float32)
    for core in range(8):
        b, r = core // 2, core % 2
        out[b, r * 1024:(r + 1) * 1024, :] = res.results[core]["outT"].T
    return out


C_AQ, C_AK, C_AV, C_BU, C_CQ, C_CK, C_CV, C_DQ, C_DK, C_DV, C_DG = (
    0, 1024, 1280, 1536, 2560, 3584, 4608, 5632, 6144, 6656, 7680)
QBLOCKS_L = [(256, 512), (768, 512), (1280, 512), (1792, 512)]


def host_consts():
    inv = 10000.0 ** (-np.arange(32, dtype=np.float32) / 32)
    tok = np.arange(NLAT)
    row = (tok // 64).astype(np.float32)
    col = (tok % 64).astype(np.float32)
    cosT = np.zeros((128, NLAT), np.float32)
    sinT = np.zeros((128, NLAT), np.float32)
    for d in range(128):
        axis = d // 64
        half = (d % 64) // 32
        f = d % 32
        ang = (row if axis == 0 else col) * inv[f]
        cosT[d] = np.cos(ang)
        sinT[d] = np.sin(ang) * (-1.0 if half == 0 else 1.0)
    pswap = np.zeros((128, 128), np.float32)
    for d in range(128):
        partner = d + 32 if (d % 64) < 32 else d - 32
        pswap[partner, d] = 1.0
    ident = np.eye(128, dtype=np.float32)
    p = np.arange(128, dtype=np.float32)
    i = np.arange(128, dtype=np.float32)
    dif = i[None, :] - p[:, None]
    rc = np.zeros((128, 2 + 128 * 6), np.float32)
    rc[:, 0] = p
    rc[:, 1] = 127 - p
    rc[:, 2:130] = np.maximum(dif, 0)
    rc[:, 130:258] = np.maximum(-dif, 0)
    rc[:, 258:386] = (dif >= 0)
    rc[:, 386:514] = (dif <= 0)
    rc[:, 514:642] = i[None, :] + 1
    rc[:, 642:770] = 128 - i[None, :]
    sel = np.zeros((8, 8 * 128), np.float32)
    for e_ in range(8):
        sel[e_, e_ * 128:(e_ + 1) * 128] = 1.0
    return dict(cosT=cosT, sinT=sinT, pswap=pswap, ident=ident, retc=rc, sel=sel)


class MixCtx:
    pass


def mix_setup(S, nc, consts_ap):
    P = S.P
    M = MixCtx()
    M.cos, M.cosb = S.sb([128, NLAT], F32, "cos")
    M.sin, M.sinb = S.sb([128, NLAT], F32, "sin")
    P.dma("sp", L("dma_start", out=M.cos[:], in_=consts_ap["cosT"]), writes=[M.cosb])
    P.dma("sp", L("dma_start", out=M.sin[:], in_=consts_ap["sinT"]), writes=[M.sinb])
    M.pswap, M.pswapb = S.sb([128, 128], BF16, "pswap")
    M.ident, M.identb = S.sb([128, 128], BF16, "ident")
    P.dma("pool", L("dma_start", out=M.pswap[:], in_=consts_ap["pswap"]), writes=[M.pswapb])
    P.dma("pool", L("dma_start", out=M.ident[:], in_=consts_ap["ident"]), writes=[M.identb])
    M.ones, M.onesb = S.sb([128, 128], BF16, "ones1")
    P.op("dve", L("memset", M.ones[:], 1.0), writes=[M.onesb])
    return M


def load_rows(S, src, srcb, r0, t0, n, name="ld", dt=BF16, pool=None):
    P = S.P
    if pool is None:
        t, tb = S.sb([128, n], dt, name)
    else:
        t, tb = S.sb_rot(name, [128, pool[1]], dt, pool[0])
    P.dma("sp", L("dma_start", out=t[:, 0:n], in_=src[r0:r0 + 128, t0:t0 + n]), reads=[srcb], writes=[tb])
    return t, tb


def rope_inplace(S, M, xt, xb, o, n, lat0, scale=None):
    P = S.P
    ps, pb = S.psum()
    P.op("pe", L("matmul", ps[:, 0:n], M.pswap[:, :], xt[:, o:o + n], start=True, stop=True),
         reads=[M.pswapb, xb], writes=[pb])
    t1, t1b = S.sb_rot("rope1", [128, 512], F32, 2)
    t2, t2b = S.sb_rot("rope2", [128, 512], F32, 2)
    P.op("dve", L("tensor_tensor", t1[:, 0:n], ps[:, 0:n], M.sin[:, lat0:lat0 + n], ALU.mult),
         reads=[pb, M.sinb], writes=[t1b])
    P.op("pool", L("tensor_tensor", t2[:, 0:n], xt[:, o:o + n], M.cos[:, lat0:lat0 + n], ALU.mult),
         reads=[xb, M.cosb], writes=[t2b])
    if scale is None:
        P.op("dve", L("tensor_tensor", xt[:, o:o + n], t1[:, 0:n], t2[:, 0:n], ALU.add),
             reads=[t1b, t2b], writes=[xb])
    else:
        P.op("dve", L("tensor_tensor", t1[:, 0:n], t1[:, 0:n], t2[:, 0:n], ALU.add),
             reads=[t1b, t2b], writes=[t1b])
        P.op("act", L("activation", xt[:, o:o + n], t1[:, 0:n], AF.Copy, scale=scale), reads=[t1b], writes=[xb])


def head_rmsnorm(S, M, xt, xb, o, n, gain, gainb):
    P = S.P
    sq, sqb = S.sb_rot("hsq", [128, 512], BF16, 2)
    P.op("act", L("activation", sq[:, 0:n], xt[:, o:o + n], AF.Square), reads=[xb], writes=[sqb])
    ps, pb = S.psum()
    P.op("pe", L("matmul", ps[:, 0:n], M.ones[:, :], sq[:, 0:n], start=True, stop=True),
         reads=[M.onesb, sqb], writes=[pb])
    rs, rsb = S.sb_rot("hrs", [128, 512], F32, 2)
    P.op("dve", L("tensor_scalar", rs[:, 0:n], ps[:, 0:n], 1.0 / 128, EPS, ALU.mult, ALU.add), reads=[pb], writes=[rsb])
    P.op("act", L("activation", rs[:, 0:n], rs[:, 0:n], AF.Ln), reads=[rsb], writes=[rsb])
    P.op("act", L("activation", rs[:, 0:n], rs[:, 0:n], AF.Exp, scale=-0.5), reads=[rsb], writes=[rsb])
    P.op("dve", L("scalar_tensor_tensor", xt[:, o:o + n], xt[:, o:o + n], gain, rs[:, 0:n], ALU.mult, ALU.mult),
         reads=[xb, rsb, gainb], writes=[xb])


def prep_qk(S, M, projT, projTb, r0, norm_gain, do_rope, tok_ranges, name, scale=None):
    P = S.P
    ntot = sum(n for _, n in tok_ranges)
    xt, xb = S.sb([128, ntot], BF16, name)
    o = 0
    for (t0, n) in tok_ranges:
        P.dma("sp", L("dma_start", out=xt[:, o:o + n], in_=projT[r0:r0 + 128, t0:t0 + n]),
              reads=[projTb], writes=[xb])
        o += n
    o = 0
    for (t0, n) in tok_ranges:
        for c0 in range(0, n, 512):
            cn = min(512, n - c0)
            if norm_gain is not None:
                head_rmsnorm(S, M, xt, xb, o + c0, cn, norm_gain[0], norm_gain[1])
            if do_rope and t0 + c0 >= NCTX:
                rope_inplace(S, M, xt, xb, o + c0, cn, t0 + c0 - NCTX, scale)
            elif scale is not None:
                P.op("act", L("activation", xt[:, o + c0:o + c0 + cn], xt[:, o + c0:o + c0 + cn], AF.Copy, scale=scale),
                     reads=[xb], writes=[xb])
        o += n
    return xt, xb


def to_tokmajor(S, M, projT, projTb, r0, ncols_tiles, name):
    P = S.P
    vt, vb = S.sb([128, T // 128, ncols_tiles * 128], BF16, name)
    for ct in range(ncols_tiles):
        for t0 in range(0, T, 512):
            n = min(512, T - t0)
            src, srcb = load_rows(S, projT, projTb, r0 + ct * 128, t0, n, "tmld", pool=(3, 512))
            for j in range(n // 128):
                ps, pb = S.psum()
                P.op("pe", L("matmul",
                    ps[:, 0:128], src[:, j * 128:(j + 1) * 128], M.ident[:, :], start=True, stop=True),
                    reads=[srcb, M.identb], writes=[pb])
                tt = t0 // 128 + j
                P.op("act", L("activation",
                    vt[:, tt, ct * 128:(ct + 1) * 128], ps[:, 0:128], AF.Copy), reads=[pb], writes=[vb])
    return vt, vb


def attn_core(S, M, qt, qb, qo, n, kt_, kb, key_tiles, vt, vb, v_col_tiles, scale, bias):
    P = S.P
    nv = len(v_col_tiles)
    accs = [S.ps[i] for i in range(nv)]
    zs = S.ps[nv]
    nk = len(key_tiles)
    for ki, ktile in enumerate(key_tiles):
        sp, spb = S.ps[4 + (ki % 4)]
        P.op("pe", L("matmul",
            sp[:, 0:n], kt_[:, ktile * 128:(ktile + 1) * 128], qt[:, qo:qo + n], start=True, stop=True),
            reads=[kb, qb], writes=[spb])
        pt, ptb = S.sb_rot("pT", [128, 512], BF16, 3)
        if bias is None:
            P.op("act", L("activation", pt[:, 0:n], sp[:, 0:n], AF.Exp, scale=scale),
                 reads=[spb], writes=[ptb])
        else:
            P.op("act", L("activation", pt[:, 0:n], sp[:, 0:n], AF.Exp, scale=scale, bias=bias[0]),
                 reads=[spb, bias[1]], writes=[ptb])
        for vi, vc in enumerate(v_col_tiles):
            a, ab = accs[vi]
            P.op("pe", L("matmul",
                a[:, 0:n], vt[:, ktile, vc * 128:(vc + 1) * 128], pt[:, 0:n], start=(ki == 0), stop=(ki == nk - 1)),
                reads=[vb, ptb], writes=[ab])
        P.op("pe", L("matmul", zs[0][:, 0:n], M.ones[:, :], pt[:, 0:n], start=(ki == 0), stop=(ki == nk - 1)),
             reads=[M.onesb, ptb], writes=[zs[1]])
    return accs, zs


def mixer_gqa(S, M, W, layer, projT, projTb, yT, yTb, need_ctx):
    P = S.P
    m = S.mark()
    S.rot = {}
    gq, gqb = S.sb([128, 1], F32, "gq")
    gk, gkb = S.sb([128, 1], F32, "gk")
    P.dma("sp", L("dma_start", out=gq[:], in_=W['attn_q_norm'][layer].rearrange("(p o) -> p o", o=1)), writes=[gqb])
    P.dma("sp", L("dma_start", out=gk[:], in_=W['attn_k_norm'][layer].rearrange("(p o) -> p o", o=1)), writes=[gkb])
    scale = 128 ** -0.5
    allr = [(0, NCTX), (NCTX, NLAT)]
    for kh in range(2):
        mm = S.mark()
        kt_, kb = prep_qk(S, M, projT, projTb, C_AK + kh * 128, (gk[:, 0:1], gkb), True, allr, "gk_k")
        vt, vb = to_tokmajor(S, M, projT, projTb, C_AV + kh * 128, 1, "gv")
        for g in range(4):
            h = kh * 4 + g
            qranges = ([(0, NCTX)] if need_ctx else []) + [(NCTX, NLAT)]
            m2 = S.mark()
            qt, qb = prep_qk(S, M, projT, projTb, C_AQ + h * 128, (gq[:, 0:1], gqb), True, qranges, "gq_q")
            qo = 0
            qlist = []
            if need_ctx:
                qlist.append((0, NCTX, [0, 1]))
            for (t0, n) in QBLOCKS_L:
                qlist.append((t0, n, list(range(T // 128))))
            for (t0, n, ktiles) in qlist:
                accs, zs = attn_core(S, M, qt, qb, qo, n, kt_, kb, ktiles, vt, vb, [0], scale, None)
                rz, rzb = S.sb_rot("rz", [128, 512], F32, 2)
                P.op("dve", L("reciprocal", rz[:, 0:n], zs[0][:, 0:n]), reads=[zs[1]], writes=[rzb])
                ot, otb = S.sb_rot("ao", [128, 512], BF16, 2)
                P.op("dve", L("tensor_tensor", ot[:, 0:n], accs[0][0][:, 0:n], rz[:, 0:n], ALU.mult),
                     reads=[accs[0][1], rzb], writes=[otb])
                P.dma("sp", L("dma_start", out=yT[h * 128:(h + 1) * 128, t0:t0 + n], in_=ot[:, 0:n]),
                      reads=[otb], writes=[yTb])
                qo += n
            S.rot_keep = None
            S.release_keep_rot(m2)
        S.release(mm)
        S.rot = {}
    S.release(m)
    S.rot = {}


def bcast_row(S, src_ap_1d, n, name):
    P = S.P
    t, tb = S.sb([128, n], F32, name)
    P.dma("sp", L("dma_start", out=t[:], in_=src_ap_1d.rearrange("(o n) -> o n", o=1).broadcast_to([128, n]),
                  allow_slow_non_contiguous=True), writes=[tb])
    return t, tb


def mixer_diff(S, M, W, layer, projT, projTb, yT, yTb, need_ctx):
    P = S.P
    m = S.mark()
    S.rot = {}
    lam_init = 0.8 - 0.6 * math.exp(-0.3 * layer)
    lp, lpb = bcast_row(S, W['diff_lambda'][layer].rearrange("a b -> (a b)"), 512, "lp")
    pr, prb = S.sb([128, 256], F32, "lpr")
    P.op("dve", L("tensor_tensor", pr[:, 0:128], lp[:, 0:128], lp[:, 128:256], ALU.mult), reads=[lpb], writes=[prb])
    P.op("dve", L("tensor_tensor", pr[:, 128:256], lp[:, 256:384], lp[:, 384:512], ALU.mult), reads=[lpb], writes=[prb])
    sm, smb = S.sb([128, 4], F32, "lsum")
    P.op("dve", L("reduce_sum", sm[:, 0:1], pr[:, 0:128], AX.X), reads=[prb], writes=[smb])
    P.op("dve", L("reduce_sum", sm[:, 1:2], pr[:, 128:256], AX.X), reads=[prb], writes=[smb])
    P.op("act", L("activation", sm[:, 0:2], sm[:, 0:2], AF.Exp), reads=[smb], writes=[smb])
    P.op("dve", L("tensor_tensor", sm[:, 2:3], sm[:, 1:2], sm[:, 0:1], ALU.subtract), reads=[smb], writes=[smb])
    P.op("dve", L("tensor_scalar", sm[:, 2:3], sm[:, 2:3], -lam_init, None, ALU.add), reads=[smb], writes=[smb])
    neglam = sm[:, 2:3]
    gn, gnb = S.sb([128, 2], F32, "dnorm")
    P.dma("sp", L("dma_start", out=gn[:], in_=W['diff_norm'][layer].rearrange("(t p) -> p t", p=128),
                  allow_slow_non_contiguous=True), writes=[gnb])
    P.op("dve", L("tensor_scalar", gn[:], gn[:], 1.0 - lam_init, None, ALU.mult), reads=[gnb], writes=[gnb])
    scale = 128 ** -0.5
    allr = [(0, NCTX), (NCTX, NLAT)]
    for h in range(4):
        mm = S.mark()
        S.rot = {}
        vt, vb = to_tokmajor(S, M, projT, projTb, C_CV + h * 256, 2, "dv")
        ks, qs, biases = [], [], []
        qranges = ([(0, NCTX)] if need_ctx else []) + [(NCTX, NLAT)]
        for mi in range(2):
            kt_, kb = prep_qk(S, M, projT, projTb, C_CK + h * 256 + mi * 128, None, True, allr, "dk%d" % mi)
            qt, qb = prep_qk(S, M, projT, projTb, C_CQ + h * 256 + mi * 128, None, True, qranges, "dq%d" % mi)
            ks.append((kt_, kb))
            qs.append((qt, qb))
            mx, mxb = S.sb([128, 4], F32, "mx%d" % mi)
            for idx, (xt, xb, ntot) in enumerate(((kt_, kb, T), (qt, qb, sum(n for _, n in qranges)))):
                part, partb = S.sb([128, 8], F32, "mxp")
                nch = 0
                for c0 in range(0, ntot, 512):
                    cn = min(512, ntot - c0)
                    sq, sqb = S.sb_rot("hsq", [128, 512], BF16, 2)
                    P.op("act", L("activation", sq[:, 0:cn], xt[:, c0:c0 + cn], AF.Square), reads=[xb], writes=[sqb])
                    ps, pb = S.psum()
                    P.op("pe", L("matmul", ps[:, 0:cn], M.ones[:, :], sq[:, 0:cn], start=True, stop=True),
                         reads=[M.onesb, sqb], writes=[pb])
                    P.op("dve", L("reduce_max", part[:, nch:nch + 1], ps[:, 0:cn], AX.X), reads=[pb], writes=[partb])
                    nch += 1
                P.op("dve", L("reduce_max", mx[:, idx:idx + 1], part[:, 0:nch], AX.X), reads=[partb], writes=[mxb])
            P.op("dve", L("tensor_tensor", mx[:, 2:3], mx[:, 0:1], mx[:, 1:2], ALU.mult), reads=[mxb], writes=[mxb])
            P.op("act", L("activation", mx[:, 2:3], mx[:, 2:3], AF.Ln), reads=[mxb], writes=[mxb])
            P.op("act", L("activation", mx[:, 2:3], mx[:, 2:3], AF.Exp, scale=0.5), reads=[mxb], writes=[mxb])
            P.op("dve", L("tensor_scalar", mx[:, 3:4], mx[:, 2:3], -scale * 1.02, None, ALU.mult), reads=[mxb], writes=[mxb])
            biases.append((mx[:, 3:4], mxb))
        qlist = []
        qo = 0
        if need_ctx:
            qlist.append((0, NCTX, [0, 1]))
        for (t0, n) in QBLOCKS_L:
            qlist.append((t0, n, list(range(T // 128))))
        for (t0, n, ktiles) in qlist:
            o_sb = []
            for mi in range(2):
                accs, zs = attn_core(S, M, qs[mi][0], qs[mi][1], qo, n, ks[mi][0], ks[mi][1], ktiles, vt, vb, [0, 1],
                                     scale, biases[mi])
                rz, rzb = S.sb_rot("rz", [128, 512], F32, 2)
                P.op("dve", L("reciprocal", rz[:, 0:n], zs[0][:, 0:n]), reads=[zs[1]], writes=[rzb])
                if mi == 1:
                    P.op("dve", L("tensor_scalar", rz[:, 0:n], rz[:, 0:n], neglam, None, ALU.mult), reads=[rzb, smb], writes=[rzb])
                for vi in range(2):
                    if mi == 0:
                        ot, otb = S.sb_rot("do%d" % vi, [128, 512], F32, 2)
                        o_sb.append((ot, otb))
                        P.op("dve", L("tensor_tensor", ot[:, 0:n], accs[vi][0][:, 0:n], rz[:, 0:n], ALU.mult),
                             reads=[accs[vi][1], rzb], writes=[otb])
                    else:
                        ot, otb = o_sb[vi]
                        tmp, tmpb = S.sb_rot("dtmp", [128, 512], F32, 2)
                        P.op("dve", L("tensor_tensor", tmp[:, 0:n], accs[vi][0][:, 0:n], rz[:, 0:n], ALU.mult),
                             reads=[accs[vi][1], rzb], writes=[tmpb])
                        P.op("pool", L("tensor_tensor", ot[:, 0:n], ot[:, 0:n], tmp[:, 0:n], ALU.add),
                             reads=[otb, tmpb], writes=[otb])
            ps, pb = S.psum()
            for vi in range(2):
                sq, sqb = S.sb_rot("hsq", [128, 512], BF16, 2)
                P.op("act", L("activation", sq[:, 0:n], o_sb[vi][0][:, 0:n], AF.Square), reads=[o_sb[vi][1]], writes=[sqb])
                P.op("pe", L("matmul", ps[:, 0:n], M.ones[:, :], sq[:, 0:n], start=(vi == 0), stop=(vi == 1)),
                     reads=[M.onesb, sqb], writes=[pb])
            rs, rsb = S.sb_rot("hrs", [128, 512], F32, 2)
            P.op("dve", L("tensor_scalar", rs[:, 0:n], ps[:, 0:n], 1.0 / 256, EPS, ALU.mult, ALU.add), reads=[pb], writes=[rsb])
            P.op("act", L("activation", rs[:, 0:n], rs[:, 0:n], AF.Ln), reads=[rsb], writes=[rsb])
            P.op("act", L("activation", rs[:, 0:n], rs[:, 0:n], AF.Exp, scale=-0.5), reads=[rsb], writes=[rsb])
            for vi in range(2):
                ob, obb = S.sb_rot("dob", [128, 512], BF16, 3)
                P.op("dve", L("scalar_tensor_tensor", ob[:, 0:n], o_sb[vi][0][:, 0:n], gn[:, vi:vi + 1], rs[:, 0:n], ALU.mult, ALU.mult),
                     reads=[o_sb[vi][1], gnb, rsb], writes=[obb])
                r0 = 2048 + h * 256 + vi * 128
                P.dma("sp", L("dma_start", out=yT[r0:r0 + 128, t0:t0 + n], in_=ob[:, 0:n]), reads=[obb], writes=[yTb])
            qo += n
        S.release(mm)
        S.rot = {}
    S.release(m)
    S.rot = {}


def mixer_ret(S, M, W, layer, projT, projTb, yT, yTb, need_ctx):
    P = S.P
    m = S.mark()
    S.rot = {}
    NT = T // 128
    kscale = 128 ** -0.5
    lg, lgb = bcast_row(S, W['ret_decay_logit'][layer].rearrange("a b -> (a b)"), 8, "lg")
    P.op("act", L("activation", lg[:], lg[:], AF.Exp, scale=-1.0), reads=[lgb], writes=[lgb])
    P.op("dve", L("tensor_scalar", lg[:], lg[:], 1.0, None, ALU.add), reads=[lgb], writes=[lgb])
    P.op("act", L("activation", lg[:], lg[:], AF.Ln), reads=[lgb], writes=[lgb])
    P.op("dve", L("tensor_scalar", lg[:], lg[:], -1.0, None, ALU.mult), reads=[lgb], writes=[lgb])
    g128, g128b = S.sb([128, 8], F32, "g128")
    P.op("act", L("activation", g128[:], lg[:], AF.Exp, scale=128.0), reads=[lgb], writes=[g128b])
    rc, rcb = S.sb([128, 770], F32, "retc")
    P.dma("sp", L("dma_start", out=rc[:], in_=S.consts["retc"]), writes=[rcb])
    pidx, pidxb = rc[:, 0:2], rcb
    pos, posb = rc[:, 2:130], rcb
    neg, negb = rc[:, 130:258], rcb
    mge, mgeb = rc[:, 258:386], rcb
    mle, mleb = rc[:, 386:514], rcb
    f1, f1b = rc[:, 514:642], rcb
    f2, f2b = rc[:, 642:770], rcb
    gn, gnb = S.sb([128, 2], F32, "rnorm")
    P.dma("sp", L("dma_start", out=gn[:], in_=W['ret_norm'][layer].rearrange("(t p) -> p t", p=128),
                  allow_slow_non_contiguous=True), writes=[gnb])
    allr = [(0, NCTX), (NCTX, NLAT)]
    qranges = ([(0, NCTX)] if need_ctx else []) + [(NCTX, NLAT)]
    qchunks = list(range(NT)) if need_ctx else list(range(2, NT))
    for h in range(4):
        mm = S.mark()
        S.rot = {}
        lf = lg[:, h:h + 1]
        lb = lg[:, 4 + h:5 + h]
        dec, decb = S.sb([128, 2], F32, "tails")
        P.op("act", L("activation", dec[:, 0:1], pidx[:, 1:2], AF.Exp, scale=lf), reads=[pidxb, lgb], writes=[decb])
        P.op("act", L("activation", dec[:, 1:2], pidx[:, 0:1], AF.Exp, scale=lb), reads=[pidxb, lgb], writes=[decb])
        qd, qdb = S.sb([128, 2, 128], F32, "qdec")
        P.op("act", L("activation", qd[:, 0, :], f1[:], AF.Exp, scale=lf), reads=[f1b, lgb], writes=[qdb])
        P.op("act", L("activation", qd[:, 1, :], f2[:], AF.Exp, scale=lb), reads=[f2b, lgb], writes=[qdb])
        Mk, Mkb = S.sb([128, 128], F32, "Mk")
        tmpm, tmpmb = S.sb([128, 128], F32, "Mtmp")
        P.op("act", L("activation", Mk[:], pos[:], AF.Exp, scale=lf), reads=[posb, lgb], writes=[Mkb])
        P.op("dve", L("tensor_tensor", Mk[:], Mk[:], mge[:], ALU.mult), reads=[Mkb, mgeb], writes=[Mkb])
        P.op("act", L("activation", tmpm[:], neg[:], AF.Exp, scale=lb), reads=[negb, lgb], writes=[tmpmb])
        P.op("dve", L("tensor_tensor", tmpm[:], tmpm[:], mle[:], ALU.mult), reads=[tmpmb, mleb], writes=[tmpmb])
        P.op("dve", L("tensor_tensor", Mk[:], Mk[:], tmpm[:], ALU.add), reads=[Mkb, tmpmb], writes=[Mkb])
        kt_, kb = prep_qk(S, M, projT, projTb, C_DK + h * 128, None, True, allr, "rk", scale=kscale)
        qt, qb = prep_qk(S, M, projT, projTb, C_DQ + h * 128, None, True, qranges, "rq")
        vt, vb = to_tokmajor(S, M, projT, projTb, C_DV + h * 256, 2, "rv")
        ktf, ktfb = S.sb([128, NT, 128], BF16, "ktf")
        ktb, ktbb = S.sb([128, NT, 128], BF16, "ktb")
        for c in range(NT):
            ps, pb = S.psum()
            P.op("pe", L("matmul", ps[:, 0:128], kt_[:, c * 128:(c + 1) * 128], M.ident[:, :], start=True, stop=True),
                 reads=[kb, M.identb], writes=[pb])
            P.op("act", L("activation", ktf[:, c, :], ps[:, 0:128], AF.Copy, scale=dec[:, 0:1]), reads=[pb, decb], writes=[ktfb])
            P.op("dve", L("tensor_scalar", ktb[:, c, :], ps[:, 0:128], dec[:, 1:2], None, ALU.mult), reads=[pb, decb], writes=[ktbb])
        sins = []
        for d, (ksc, kscb, order) in enumerate(((ktf, ktfb, list(range(NT))), (ktb, ktbb, [1, 0] + list(range(NT - 1, 1, -1))))):
            Sst, Sstb = S.sb([128, 256], F32, "Sst%d" % d)
            P.op("dve", L("memset", Sst[:], 0.0), writes=[Sstb])
            Sin, Sinb = S.sb([128, NT, 256], BF16, "Sin%d" % d)
            gcol = g128[:, d * 4 + h:d * 4 + h + 1]
            for c in order:
                P.op("act", L("activation", Sin[:, c, :], Sst[:], AF.Copy), reads=[Sstb], writes=[Sinb])
                ps, pb = S.psum()
                P.op("pe", L("matmul", ps[:, 0:256], ksc[:, c, :], vt[:, c, :], start=True, stop=True),
                     reads=[kscb, vb], writes=[pb])
                P.op("dve", L("scalar_tensor_tensor", Sst[:], Sst[:], gcol, ps[:, 0:256], ALU.mult, ALU.add),
                     reads=[Sstb, g128b, pb], writes=[Sstb])
            sins.append((Sin, Sinb))
        for ci, c in enumerate(qchunks):
            qc = ci * 128
            ps, pb = S.psum()
            P.op("pe", L("matmul", ps[:, 0:128], kt_[:, c * 128:(c + 1) * 128], qt[:, qc:qc + 128], start=True, stop=True),
                 reads=[kb, qb], writes=[pb])
            stm, stmb = S.sb_rot("stm", [128, 128], BF16, 3)
            P.op("dve", L("tensor_tensor", stm[:], ps[:, 0:128], Mk[:], ALU.mult), reads=[pb, Mkb], writes=[stmb])
            qf, qfb = S.sb_rot("qf", [128, 2, 128], BF16, 3)
            P.op("pool", L("tensor_tensor", qf[:, 0, :], qt[:, qc:qc + 128], qd[:, 0, :], ALU.mult), reads=[qb, qdb], writes=[qfb])
            P.op("pool", L("tensor_tensor", qf[:, 1, :], qt[:, qc:qc + 128], qd[:, 1, :], ALU.mult), reads=[qb, qdb], writes=[qfb])
            os_ = []
            for et in range(2):
                po, pob = S.psum()
                P.op("pe", L("matmul", po[:, 0:128], vt[:, c, et * 128:(et + 1) * 128], stm[:], start=True, stop=False),
                     reads=[vb, stmb], writes=[pob])
                P.op("pe", L("matmul", po[:, 0:128], sins[0][0][:, c, et * 128:(et + 1) * 128], qf[:, 0, :], start=False, stop=False),
                     reads=[sins[0][1], qfb], writes=[pob])
                P.op("pe", L("matmul", po[:, 0:128], sins[1][0][:, c, et * 128:(et + 1) * 128], qf[:, 1, :], start=False, stop=True),
                     reads=[sins[1][1], qfb], writes=[pob])
                osb, osbb = S.sb_rot("ro%d" % et, [128, 128], F32, 2)
                P.op("act", L("activation", osb[:], po[:, 0:128], AF.Copy), reads=[pob], writes=[osbb])
                os_.append((osb, osbb))
            ps2, pb2 = S.psum()
            for et in range(2):
                sq, sqb = S.sb_rot("rsq", [128, 128], BF16, 2)
                P.op("act", L("activation", sq[:], os_[et][0][:], AF.Square), reads=[os_[et][1]], writes=[sqb])
                P.op("pe", L("matmul", ps2[:, 0:128], M.ones[:, :], sq[:], start=(et == 0), stop=(et == 1)),
                     reads=[M.onesb, sqb], writes=[pb2])
            rs, rsb = S.sb_rot("rrs", [128, 128], F32, 2)
            P.op("dve", L("tensor_scalar", rs[:], ps2[:, 0:128], 1.0 / 256, EPS, ALU.mult, ALU.add), reads=[pb2], writes=[rsb])
            P.op("act", L("activation", rs[:], rs[:], AF.Ln), reads=[rsb], writes=[rsb])
            P.op("act", L("activation", rs[:], rs[:], AF.Exp, scale=-0.5), reads=[rsb], writes=[rsb])
            t0 = c * 128
            for et in range(2):
                gt_, gtb = load_rows(S, projT, projTb, C_DG + h * 256 + et * 128, t0, 128, "rgate", pool=(3, 128))
                sg, sgb = S.sb_rot("rsg", [128, 128], F32, 2)
                P.op("act", L("activation", sg[:], gt_[:, 0:128], AF.Silu), reads=[gtb], writes=[sgb])
                P.op("dve", L("scalar_tensor_tensor", os_[et][0][:], os_[et][0][:], gn[:, et:et + 1], rs[:], ALU.mult, ALU.mult),
                     reads=[os_[et][1], gnb, rsb], writes=[os_[et][1]])
                ob, obb = S.sb_rot("rob", [128, 128], BF16, 3)
                P.op("dve", L("tensor_tensor", ob[:], os_[et][0][:], sg[:], ALU.mult), reads=[os_[et][1], sgb], writes=[obb])
                r0 = 3072 + h * 256 + et * 128
                P.dma("sp", L("dma_start", out=yT[r0:r0 + 128, t0:t0 + 128], in_=ob[:]), reads=[obb], writes=[yTb])
        S.release(mm)
        S.rot = {}
    S.release(m)
    S.rot = {}


def mixer_s5(S, M, W, layer, projT, projTb, yT, yTb, need_ctx):
    P = S.P
    nc = S.nc
    m = S.mark()
    S.rot = {}
    MAGIC = 12582912.0
    C1 = 6.28125
    C2 = 2 * math.pi - C1
    yF = S.dram.get("s5yF")
    if yF is None:
        yF = S.dt("s5yF", [512, T], F32)
        S.dt("s5yB", [512, T], F32)
        for ch_ in range(4):
            S.dt("s5ysc%d" % ch_, [128, T], F32)
            S.dt("s5ygc%d" % ch_, [256, T], F32)
    yB = S.dram["s5yB"]
    yFb, yBb = S.dbuf["s5yF"], S.dbuf["s5yB"]
    tix, tixb = S.sb([128, T], F32, "tix")
    P.op("pool", L("iota", tix[:], [[1, T]], base=0, channel_multiplier=0, allow_small_or_imprecise_dtypes=True), writes=[tixb])
    onesT, onesTb = S.sb([128, T], F32, "onesT")
    P.op("dve", L("memset", onesT[:], 1.0), writes=[onesTb])

    def sincos(dst_sin, dst_cos, ang, angb, n, tmp, tmpb, dstb):
        for (dst, shift) in ((dst_sin, 0.0), (dst_cos, math.pi / 2)):
            P.op("dve", L("tensor_scalar", tmp[0][:, 0:n], ang, shift, 1.0 / (2 * math.pi), ALU.add, ALU.mult), reads=[angb], writes=[tmpb])
            P.op("dve", L("tensor_scalar", tmp[0][:, 0:n], tmp[0][:, 0:n], MAGIC, MAGIC, ALU.add, ALU.subtract), reads=[tmpb], writes=[tmpb])
            P.op("dve", L("tensor_scalar", tmp[1][:, 0:n], ang, shift, None, ALU.add), reads=[angb], writes=[tmpb])
            P.op("dve", L("scalar_tensor_tensor", tmp[1][:, 0:n], tmp[0][:, 0:n], -C1, tmp[1][:, 0:n], ALU.mult, ALU.add), reads=[tmpb], writes=[tmpb])
            P.op("dve", L("scalar_tensor_tensor", tmp[1][:, 0:n], tmp[0][:, 0:n], -C2, tmp[1][:, 0:n], ALU.mult, ALU.add), reads=[tmpb], writes=[tmpb])
            P.op("act", L("activation", dst, tmp[1][:, 0:n], AF.Sin), reads=[tmpb], writes=[dstb])

    dsk, dskb = S.sb([128, 8], F32, "dskip")
    P.dma("sp", L("dma_start", out=dsk[:], in_=W['s5_d'][layer].rearrange("(t p) -> p t", p=128), allow_slow_non_contiguous=True), writes=[dskb])
    for d in range(2):
        md = S.mark()
        S.rot = {}
        are, areb = S.sb([128, 32], F32, "are")
        aim, aimb = S.sb([128, 32], F32, "aim")
        P.dma("sp", L("dma_start", out=are[:], in_=W['s5_a_re'][layer, d].rearrange("(j h) n -> (h n) j", h=2), allow_slow_non_contiguous=True), writes=[areb])
        P.dma("sp", L("dma_start", out=aim[:], in_=W['s5_a_im'][layer, d].rearrange("(j h) n -> (h n) j", h=2), allow_slow_non_contiguous=True), writes=[aimb])
        st_, stb = S.sb([128, 32], F32, "step")
        lsv = W['s5_log_step'][layer, d].rearrange("(j h) -> h j", h=2)
        for h in range(2):
            P.dma("sp", L("dma_start", out=st_[64 * h:64 * h + 64, :], in_=lsv[h:h + 1, :].broadcast_to([64, 32]), allow_slow_non_contiguous=True), writes=[stb])
        P.op("act", L("activation", st_[:], st_[:], AF.Exp), reads=[stb], writes=[stb])
        pr, prb = S.sb([128, 12, 32], F32, "s5par")
        P.op("dve", L("tensor_tensor", pr[:, 0, :], are[:], st_[:], ALU.mult), reads=[areb, stb], writes=[prb])
        P.op("act", L("activation", pr[:, 0, :], pr[:, 0, :], AF.Exp), reads=[prb], writes=[prb])
        P.op("dve", L("tensor_tensor", pr[:, 1, :], aim[:], st_[:], ALU.mult), reads=[aimb, stb], writes=[prb])
        tmpA, tmpAb = S.sb([128, 32], F32, "tA")
        tmpB, tmpBb = S.sb([128, 32], F32, "tB")
        sincos(pr[:, 2, :], pr[:, 3, :], pr[:, 1, :], prb, 32, (tmpA, tmpB), tmpAb, prb)
        P.op("dve", L("tensor_tensor", pr[:, 4, :], pr[:, 0, :], pr[:, 3, :], ALU.mult), reads=[prb], writes=[prb])
        P.op("dve", L("tensor_scalar", pr[:, 4, :], pr[:, 4, :], -1.0, None, ALU.add), reads=[prb], writes=[prb])
        P.op("dve", L("tensor_tensor", pr[:, 5, :], pr[:, 0, :], pr[:, 2, :], ALU.mult), reads=[prb], writes=[prb])
        P.op("dve", L("tensor_tensor", pr[:, 9, :], are[:], are[:], ALU.mult), reads=[areb], writes=[prb])
        P.op("dve", L("tensor_tensor", pr[:, 10, :], aim[:], aim[:], ALU.mult), reads=[aimb], writes=[prb])
        P.op("dve", L("tensor_tensor", pr[:, 8, :], pr[:, 9, :], pr[:, 10, :], ALU.add), reads=[prb], writes=[prb])
        P.op("dve", L("reciprocal", pr[:, 8, :], pr[:, 8, :]), reads=[prb], writes=[prb])
        P.op("dve", L("tensor_tensor", pr[:, 9, :], pr[:, 4, :], are[:], ALU.mult), reads=[prb, areb], writes=[prb])
        P.op("dve", L("tensor_tensor", pr[:, 10, :], pr[:, 5, :], aim[:], ALU.mult), reads=[prb, aimb], writes=[prb])
        P.op("dve", L("tensor_tensor", pr[:, 6, :], pr[:, 9, :], pr[:, 10, :], ALU.add), reads=[prb], writes=[prb])
        P.op("dve", L("tensor_tensor", pr[:, 6, :], pr[:, 6, :], pr[:, 8, :], ALU.mult), reads=[prb], writes=[prb])
        P.op("dve", L("tensor_tensor", pr[:, 9, :], pr[:, 5, :], are[:], ALU.mult), reads=[prb, areb], writes=[prb])
        P.op("dve", L("tensor_tensor", pr[:, 10, :], pr[:, 4, :], aim[:], ALU.mult), reads=[prb, aimb], writes=[prb])
        P.op("dve", L("tensor_tensor", pr[:, 7, :], pr[:, 9, :], pr[:, 10, :], ALU.subtract), reads=[prb], writes=[prb])
        P.op("dve", L("tensor_tensor", pr[:, 7, :], pr[:, 7, :], pr[:, 8, :], ALU.mult), reads=[prb], writes=[prb])
        rp, rpb = S.sb([128, 12, 32], F32, "rpow")
        P.op("dve", L("tensor_copy", rp[:, 0, :], pr[:, 0, :]), reads=[prb], writes=[rpb])
        for k in range(1, 12):
            P.op("dve", L("tensor_tensor", rp[:, k, :], rp[:, k - 1, :], rp[:, k - 1, :], ALU.mult), reads=[rpb], writes=[rpb])
        Cr, Crb = S.sb([128, 32, 16], F32, "Cr")
        Ci, Cib = S.sb([128, 32, 16], F32, "Ci")
        for (Ct, Cb_, key) in ((Cr, Crb, 's5_c_re'), (Ci, Cib, 's5_c_im')):
            csrc = W[key][layer, d]
            for h in range(2):
                for j_ in range(32):
                    P.dma("sp", L("dma_start", out=Ct[64 * h:64 * h + 64, j_, :], in_=csrc[2 * j_ + h].rearrange("p n -> n p"), allow_slow_non_contiguous=True), writes=[Cb_])
        Cbd = [S.sb([128, 32, 32], BF16, "Cbd%d" % i) for i in range(2)]
        for i in range(2):
            P.op("dve", L("memset", Cbd[i][0][:], 0.0), writes=[Cbd[i][1]])
        t16a, t16ab = S.sb([128, 32], F32, "t16a")
        t16b, t16bb = S.sb([128, 32], F32, "t16b")
        for p_ in range(16):
            P.op("dve", L("tensor_tensor", t16a[:], Cr[:, :, p_], pr[:, 6, :], ALU.mult), reads=[Crb, prb], writes=[t16ab])
            P.op("dve", L("tensor_tensor", t16b[:], Ci[:, :, p_], pr[:, 7, :], ALU.mult), reads=[Cib, prb], writes=[t16bb])
            for h in range(2):
                P.op("dve", L("tensor_tensor", Cbd[0][0][64 * h:64 * h + 64, :, 16 * h + p_], t16a[64 * h:64 * h + 64, :], t16b[64 * h:64 * h + 64, :], ALU.subtract),
                     reads=[t16ab, t16bb], writes=[Cbd[0][1]])
            P.op("dve", L("tensor_tensor", t16a[:], Cr[:, :, p_], pr[:, 7, :], ALU.mult), reads=[Crb, prb], writes=[t16ab])
            P.op("dve", L("tensor_tensor", t16b[:], Ci[:, :, p_], pr[:, 6, :], ALU.mult), reads=[Cib, prb], writes=[t16bb])
            P.op("dve", L("tensor_tensor", t16a[:], t16a[:], t16b[:], ALU.add), reads=[t16ab, t16bb], writes=[t16ab])
            for h in range(2):
                P.op("dve", L("tensor_scalar", Cbd[1][0][64 * h:64 * h + 64, :, 16 * h + p_], t16a[64 * h:64 * h + 64, :], -1.0, None, ALU.mult),
                     reads=[t16ab], writes=[Cbd[1][1]])
        Bbd = []
        for key in ('s5_b_re', 's5_b_im'):
            stg, stgb = S.sb([128, 8, 4, 128], F32, "Bstg")
            P.op("pool", L("memset", stg[:], 0.0), writes=[stgb])
            bsrc = W[key][layer, d]
            for ct in range(8):
                for jj in range(4):
                    for h in range(2):
                        g = 8 * ct + 2 * jj + h
                        P.dma("sp", L("dma_start", out=stg[32 * jj + 16 * h:32 * jj + 16 * h + 16, ct, jj, 64 * h:64 * h + 64],
                                      in_=bsrc[g].rearrange("n q -> q n"), allow_slow_non_contiguous=True), writes=[stgb])
            bb_, bbb = S.sb([128, 8, 4, 128], BF16, "Bbd")
            P.op("act", L("activation", bb_[:], stg[:], AF.Copy), reads=[stgb], writes=[bbb])
            Bbd.append((bb_, bbb))
        segs = [(0, NCTX, 0), (NCTX, NLAT, NCTX)] if d == 0 else [(NCTX, NLAT, 0), (0, NCTX, NLAT)]
        sel, selb = S.psel
        for ct in range(4):
            mc = S.mark()
            S.rot = {}
            ut, utb = S.sb([128, T], BF16, "s5u")
            ut2, ut2b = S.sb([128, T], BF16, "s5u2")
            for (t0, n, c0) in segs:
                P.dma("sp", L("dma_start", out=ut[:, c0:c0 + n], in_=projT[C_BU + ct * 128:C_BU + (ct + 1) * 128, t0:t0 + n]), reads=[projTb], writes=[utb])
                P.dma("sp", L("dma_start", out=ut2[:, c0:c0 + n], in_=projT[C_BU + (ct + 4) * 128:C_BU + (ct + 5) * 128, t0:t0 + n]), reads=[projTb], writes=[ut2b])
            P.op("dve", L("tensor_scalar", ut[:], ut[:], sel[:, 0:1], None, ALU.mult), reads=[utb, selb], writes=[utb])
            P.op("dve", L("scalar_tensor_tensor", ut[:], ut2[:], sel[:, 1:2], ut[:], ALU.mult, ALU.add), reads=[ut2b, selb, utb], writes=[utb])
            for jj in range(4):
                j0_, j1_ = 4 * ct + jj, 16 + 4 * ct + jj
                k0 = 64 * (jj // 2)
                bs_ = []
                for i_ in range(2):
                    bt_, btb_ = S.sb_rot("bsel%d" % i_, [128, 128], BF16, 2)
                    P.op("dve", L("tensor_scalar", bt_[k0:k0 + 64, :], Bbd[i_][0][k0:k0 + 64, ct, jj, :], sel[k0:k0 + 64, 0:1], None, ALU.mult),
                         reads=[Bbd[i_][1], selb], writes=[btb_])
                    P.op("dve", L("scalar_tensor_tensor", bt_[k0:k0 + 64, :], Bbd[i_][0][k0:k0 + 64, ct + 4, jj, :], sel[k0:k0 + 64, 1:2],
                                  bt_[k0:k0 + 64, :], ALU.mult, ALU.add), reads=[Bbd[i_][1], selb, btb_], writes=[btb_])
                    bs_.append((bt_, btb_))
                cs_ = []
                for i_ in range(2):
                    ctile, ctileb = S.sb_rot("csel%d" % i_, [128, 32], BF16, 2)
                    P.op("dve", L("tensor_scalar", ctile[:], Cbd[i_][0][:, j0_, :], sel[:, 0:1], None, ALU.mult), reads=[Cbd[i_][1], selb], writes=[ctileb])
                    P.op("dve", L("scalar_tensor_tensor", ctile[:], Cbd[i_][0][:, j1_, :], sel[:, 1:2], ctile[:], ALU.mult, ALU.add),
                         reads=[Cbd[i_][1], selb, ctileb], writes=[ctileb])
                    cs_.append((ctile, ctileb))
                sc, scb = S.sb_rot("scsel", [128, 16], F32, 2)
                for (col, srcA, srcB, sb_) in ([(0, pr[:, 1, j0_:j0_ + 1], pr[:, 1, j1_:j1_ + 1], prb), (1, pr[:, 0, j0_:j0_ + 1], pr[:, 0, j1_:j1_ + 1], prb)]
                                               + [(2 + k_, rp[:, k_, j0_:j0_ + 1], rp[:, k_, j1_:j1_ + 1], rpb) for k_ in range(12)]):
                    P.op("dve", L("tensor_scalar", sc[:, col:col + 1], srcA, sel[:, 0:1], None, ALU.mult), reads=[sb_, selb], writes=[scb])
                    P.op("dve", L("scalar_tensor_tensor", sc[:, col:col + 1], srcB, sel[:, 1:2], sc[:, col:col + 1], ALU.mult, ALU.add),
                         reads=[sb_, selb, scb], writes=[scb])
                xr, xrb = S.sb_rot("xr", [128, T], F32, 1)
                xi, xib = S.sb_rot("xi", [128, T], F32, 1)
                cs, csb = S.sb_rot("cs", [128, T], F32, 1)
                sn, snb = S.sb_rot("sn", [128, T], F32, 1)
                ang, angb = S.sb_rot("ang", [128, T], F32, 1)
                tA, tAb = S.sb_rot("tAA", [128, T], F32, 1)
                tB, tBb = S.sb_rot("tBB", [128, T], F32, 1)
                P.op("pool", L("tensor_scalar", ang[:], tix[:], sc[:, 0:1], None, ALU.mult), reads=[tixb, scb], writes=[angb])
                sincos(sn[:], cs[:], ang[:], angb, T, (tA, tB), tAb, snb)
                sgn = 1.0 if d == 0 else -1.0
                for c0 in range(0, T, 512):
                    n = min(512, T - c0)
                    pre, preb = S.psum()
                    pim, pimb = S.psum()
                    P.op("pe", L("matmul", pre[:, 0:n], bs_[0][0][k0:k0 + 64, :], ut[k0:k0 + 64, c0:c0 + n], start=True, stop=True),
                         reads=[bs_[0][1], utb], writes=[preb])
                    P.op("pe", L("matmul", pim[:, 0:n], bs_[1][0][k0:k0 + 64, :], ut[k0:k0 + 64, c0:c0 + n], start=True, stop=True),
                         reads=[bs_[1][1], utb], writes=[pimb])
                    P.op("dve", L("tensor_tensor", tA[:, c0:c0 + n], pre[:, 0:n], cs[:, c0:c0 + n], ALU.mult), reads=[preb, snb], writes=[tAb])
                    P.op("dve", L("tensor_tensor", tB[:, c0:c0 + n], pim[:, 0:n], sn[:, c0:c0 + n], ALU.mult), reads=[pimb, snb], writes=[tBb])
                    P.op("dve", L("scalar_tensor_tensor", xr[:, c0:c0 + n], tB[:, c0:c0 + n], sgn, tA[:, c0:c0 + n], ALU.mult, ALU.add), reads=[tAb, tBb], writes=[xrb])
                    P.op("dve", L("tensor_tensor", tA[:, c0:c0 + n], pim[:, 0:n], cs[:, c0:c0 + n], ALU.mult), reads=[pimb, snb], writes=[tAb])
                    P.op("dve", L("tensor_tensor", tB[:, c0:c0 + n], pre[:, 0:n], sn[:, c0:c0 + n], ALU.mult), reads=[preb, snb], writes=[tBb])
                    P.op("dve", L("scalar_tensor_tensor", xi[:, c0:c0 + n], tB[:, c0:c0 + n], -sgn, tA[:, c0:c0 + n], ALU.mult, ALU.add), reads=[tAb, tBb], writes=[xib])
                if d == 0:
                    P.op("act", L("activation", tA[:], onesT[:], AF.Copy, scale=sc[:, 1:2]), reads=[onesTb, scb], writes=[tAb])
                    P.op("dve", L("tensor_tensor_scan", xr[:], tA[:], xr[:], 0.0, ALU.mult, ALU.add), reads=[tAb, xrb], writes=[xrb])
                    P.op("dve", L("tensor_tensor_scan", xi[:], tA[:], xi[:], 0.0, ALU.mult, ALU.add), reads=[tAb, xib], writes=[xib])
                else:
                    for k in range(12):
                        s_ = 1 << k
                        if s_ >= T:
                            break
                        for (xx, xxb, eng) in ((xr, xrb, "dve"), (xi, xib, "dve")):
                            P.op(eng, L("scalar_tensor_tensor", xx[:, 0:T - s_], xx[:, s_:T], sc[:, 2 + k:3 + k], xx[:, 0:T - s_], ALU.mult, ALU.add),
                                 reads=[xxb, scb], writes=[xxb])
                Xr, Xrb = S.sb_rot("Xr", [128, T], BF16, 1)
                Xi, Xib = S.sb_rot("Xi", [128, T], BF16, 1)
                P.op("dve", L("tensor_tensor", tA[:], xr[:], cs[:], ALU.mult), reads=[xrb, snb], writes=[tAb])
                P.op("pool", L("tensor_tensor", tB[:], xi[:], sn[:], ALU.mult), reads=[xib, snb], writes=[tBb])
                P.op("dve", L("scalar_tensor_tensor", Xr[:], tB[:], -sgn, tA[:], ALU.mult, ALU.add), reads=[tAb, tBb], writes=[Xrb])
                P.op("dve", L("tensor_tensor", tA[:], xi[:], cs[:], ALU.mult), reads=[xib, snb], writes=[tAb])
                P.op("pool", L("tensor_tensor", tB[:], xr[:], sn[:], ALU.mult), reads=[xrb, snb], writes=[tBb])
                P.op("dve", L("scalar_tensor_tensor", Xi[:], tB[:], sgn, tA[:], ALU.mult, ALU.add), reads=[tAb, tBb], writes=[Xib])
                ydst, ydstb = (yF, yFb) if d == 0 else (yB, yBb)
                for (t0, n, c0) in segs:
                    for cc0 in range(0, n, 512):
                        cn = min(512, n - cc0)
                        po, pob = S.psum()
                        P.op("pe", L("matmul", po[0:32, 0:cn], cs_[0][0][:, :], Xr[:, c0 + cc0:c0 + cc0 + cn], start=True, stop=False),
                             reads=[cs_[0][1], Xrb], writes=[pob])
                        P.op("pe", L("matmul", po[0:32, 0:cn], cs_[1][0][:, :], Xi[:, c0 + cc0:c0 + cc0 + cn], start=False, stop=True),
                             reads=[cs_[1][1], Xib], writes=[pob])
                        yo, yob = S.sb_rot("s5yo", [32, 512], F32, 3)
                        P.op("act", L("activation", yo[:, 0:cn], po[0:32, 0:cn], AF.Copy), reads=[pob], writes=[yob])
                        r0 = ct * 128 + 32 * jj
                        P.dma("sp", L("dma_start", out=ydst[r0:r0 + 32, t0 + cc0:t0 + cc0 + cn], in_=yo[:, 0:cn]), reads=[yob], writes=[ydstb])
            S.release(mc)
            S.rot = {}
        S.release(md)
        S.rot = {}
    tok0 = 0 if need_ctx else NCTX
    ntok = T - tok0
    ygb = Buf("s5yg")
    ncc = S.ncores // 2
    for ch_ in range(4):
        ysc, yscb = S.dram["s5ysc%d" % ch_], S.dbuf["s5ysc%d" % ch_]
        ygc = S.dram["s5ygc%d" % ch_]
        for c0 in range(0, T, 1152):
            a_, ab_ = S.sb_rot("ga", [128, 1152], F32, 2)
            b_, bb_ = S.sb_rot("gb", [128, 1152], F32, 2)
            P.dma("sp", L("dma_start", out=a_[:], in_=yF[ch_ * 128:(ch_ + 1) * 128, c0:c0 + 1152]), reads=[yFb], writes=[ab_])
            P.dma("sp", L("dma_start", out=b_[:], in_=yB[ch_ * 128:(ch_ + 1) * 128, c0:c0 + 1152]), reads=[yBb], writes=[bb_])
            P.op("dve", L("tensor_tensor", a_[:], a_[:], b_[:], ALU.add), reads=[ab_, bb_], writes=[ab_])
            P.dma("sp", L("dma_start", out=ysc[:, c0:c0 + 1152], in_=a_[:]), reads=[ab_], writes=[yscb])
        P.coll(L("collective_compute", "AllGather", ALU.bypass, replica_groups=[[2 * i, 2 * i + 1] for i in range(ncc)],
                 ins=[ysc.opt()], outs=[ygc.opt()]), reads=[yscb], writes=[ygb])
    zt, ztb = S.sb([128, 8, T], BF16, "s5z")
    for ct in range(8):
        ygc = S.dram["s5ygc%d" % (ct % 4)]
        rr_ = ct // 4
        for c0 in range(tok0, T, 512):
            n = min(512, T - c0)
            a_, ab_ = S.sb_rot("fa", [128, 512], F32, 2)
            b_, bb_ = S.sb_rot("fb", [128, 512], F32, 2)
            u_, ub_ = S.sb_rot("fu", [128, 512], BF16, 2)
            P.dma("sp", L("dma_start", out=a_[:, 0:n], in_=ygc[rr_ * 128:(rr_ + 1) * 128, c0:c0 + n]), reads=[ygb], writes=[ab_])
            P.dma("sp", L("dma_start", out=u_[:, 0:n], in_=projT[C_BU + ct * 128:C_BU + (ct + 1) * 128, c0:c0 + n]), reads=[projTb], writes=[ub_])
            P.op("dve", L("scalar_tensor_tensor", a_[:, 0:n], u_[:, 0:n], dsk[:, ct:ct + 1], a_[:, 0:n], ALU.mult, ALU.add), reads=[ub_, dskb, ab_], writes=[ab_])
            P.op("dve", L("tensor_tensor", b_[:, 0:n], a_[:, 0:n], a_[:, 0:n], ALU.mult), reads=[ab_], writes=[bb_])
            P.op("dve", L("tensor_scalar", b_[:, 0:n], b_[:, 0:n], 0.044715, 1.0, ALU.mult, ALU.add), reads=[bb_], writes=[bb_])
            P.op("dve", L("tensor_tensor", b_[:, 0:n], b_[:, 0:n], a_[:, 0:n], ALU.mult), reads=[bb_, ab_], writes=[bb_])
            P.op("act", L("activation", b_[:, 0:n], b_[:, 0:n], AF.Tanh, scale=0.7978845608028654), reads=[bb_], writes=[bb_])
            P.op("dve", L("tensor_scalar", b_[:, 0:n], b_[:, 0:n], 1.0, 0.5, ALU.add, ALU.mult), reads=[bb_], writes=[bb_])
            P.op("dve", L("tensor_tensor", zt[:, ct, c0:c0 + n], b_[:, 0:n], a_[:, 0:n], ALU.mult), reads=[bb_, ab_], writes=[ztb])
    blocks = [(c0, min(512, T - c0)) for c0 in range(tok0, T, 512)]
    offs = {bi: blocks[bi][0] for bi in range(len(blocks))}

    def epi(ti, mt, b, tn, ps, pb):
        t0, n = tn
        sg, sgb = S.sb_rot("gsig", [128, 512], F32, 2)
        P.op("act", L("activation", sg[:, 0:n], ps[:, 0:n], AF.Sigmoid), reads=[pb], writes=[sgb])
        ob, obb = S.sb_rot("gob", [128, 512], BF16, 3)
        P.op("dve", L("tensor_tensor", ob[:, 0:n], sg[:, 0:n], zt[:, mt, t0:t0 + n], ALU.mult), reads=[sgb, ztb], writes=[obb])
        P.dma("sp", L("dma_start", out=yT[1024 + mt * 128:1024 + (mt + 1) * 128, t0:t0 + n], in_=ob[:, 0:n]), reads=[obb], writes=[yTb])
    wg = W['s5_w_glu'][layer]
    stream_gemm(S, [dict(w=lambda mt: wg[:, mt * 128:(mt + 1) * 128], nkt=8, src=(zt, ztb, offs))], 8,
                list(range(len(blocks))), blocks, epi, wbufs=3)
    S.release(m)
    S.rot = {}
```
